# Optimizing a Trainium2 kernel written in Bass

```python
import jax
import jax.numpy as jnp
from jax import lax
import numpy as np

D_MODEL = 2048
BATCH = 4
SEQ = 8192
DEPTH = 1

GRID_W = 64
CTX_LEN = 256

ATTN_HEADS = 16
ATTN_KV_HEADS = 4
ATTN_GROUP = ATTN_HEADS // ATTN_KV_HEADS
HEAD_DIM = 128
WINDOW = 128
BLOCK = 128
ROPE_BASE = 10000.0

MLSTM_HEADS = 8
MLSTM_QK_DIM = 128
MLSTM_V_DIM = 256
MLSTM_CHUNK = 128
N_DIRS = 2
FGATE_BIAS_LO = 3.0
FGATE_BIAS_HI = 6.0

D_FF = -(-8 * D_MODEL // (3 * 256)) * 256

NORM_EPS = 1e-6
N_MOD = 6

ATTN_Q_W = ATTN_HEADS * HEAD_DIM
ATTN_KV_W = ATTN_KV_HEADS * HEAD_DIM
MLSTM_QK_W = MLSTM_HEADS * MLSTM_QK_DIM
MLSTM_V_W = MLSTM_HEADS * MLSTM_V_DIM
N_GATE = N_DIRS * 2 * MLSTM_HEADS
KV_SIDE_SIZES = (ATTN_KV_W, ATTN_KV_W, MLSTM_QK_W, MLSTM_V_W, N_GATE)
Q_SIDE_SIZES = (ATTN_Q_W, MLSTM_QK_W, MLSTM_V_W, D_MODEL, D_MODEL)
N_KV_SIDE = sum(KV_SIDE_SIZES)
N_IN = N_KV_SIDE + sum(Q_SIDE_SIZES)

kernel_name = "hybrid_dit_gqa_mlstm_prefix"


def _split(p, sizes):
    idx = [int(i) for i in np.cumsum(sizes)[:-1]]
    return jnp.split(p, idx, axis=-1)


def rms_norm(x, g):
    xf = x.astype(jnp.float32)
    y = xf * lax.rsqrt(jnp.mean(xf * xf, axis=-1, keepdims=True) + NORM_EPS)
    return (y * g.astype(jnp.float32)).astype(x.dtype)


def modulate(h, shift, scale):
    return h * (1 + scale) + shift


def adaln_params(cvec, w, b, n_chunks):
    mod = jax.nn.silu(cvec) @ w[:, :n_chunks * D_MODEL] + b[:n_chunks * D_MODEL]
    return jnp.split(mod, n_chunks, axis=-1)


def _rope_1d(x, pos):
    half = x.shape[-1] // 2
    inv_freq = ROPE_BASE ** (-jnp.arange(half, dtype=jnp.float32) / half)
    ang = pos.astype(jnp.float32)[:, None] * inv_freq[None, :]
    cos = jnp.cos(ang)[:, None, :]
    sin = jnp.sin(ang)[:, None, :]
    x1 = x[..., :half].astype(jnp.float32)
    x2 = x[..., half:].astype(jnp.float32)
    return jnp.concatenate([x1 * cos - x2 * sin, x2 * cos + x1 * sin], axis=-1)


def rope_2d(x, rows, cols):
    half = x.shape[-1] // 2
    y = jnp.concatenate([_rope_1d(x[..., :half], rows), _rope_1d(x[..., half:], cols)], axis=-1)
    return y.astype(x.dtype)


def window_attention(q, k, v, kc, vc, sink):
    B, S = q.shape[:2]
    C = kc.shape[1]
    nb = S // BLOCK
    scale = HEAD_DIM ** -0.5
    qb = q.reshape(B, nb, BLOCK, ATTN_KV_HEADS, ATTN_GROUP, HEAD_DIM)

    def band(t):
        tp = jnp.pad(t, ((0, 0), (BLOCK, BLOCK), (0, 0), (0, 0)))
        tp = tp.reshape(B, nb + 2, BLOCK, ATTN_KV_HEADS, HEAD_DIM)
        return jnp.concatenate([tp[:, :-2], tp[:, 1:-1], tp[:, 2:]], axis=2)

    kb, vb = band(k), band(v)
    s_loc = jnp.einsum("bnqhgd,bnkhd->bhgnqk", qb, kb).astype(jnp.float32) * scale
    s_ctx = jnp.einsum("bnqhgd,bchd->bhgnqc", qb, kc).astype(jnp.float32) * scale
    qpos = jnp.arange(S).reshape(nb, BLOCK, 1)
    kpos = (jnp.arange(nb) * BLOCK - BLOCK)[:, None, None] + jnp.arange(3 * BLOCK)[None, None, :]
    valid = (jnp.abs(qpos - kpos) <= WINDOW) & (kpos >= 0) & (kpos < S)
    s_loc = jnp.where(valid, s_loc, -jnp.inf)
    sink_l = jnp.broadcast_to(sink.astype(jnp.float32).reshape(1, ATTN_KV_HEADS, ATTN_GROUP, 1, 1, 1),
                              s_loc.shape[:-1] + (1,))
    p = jax.nn.softmax(jnp.concatenate([s_loc, s_ctx, sink_l], axis=-1), axis=-1).astype(v.dtype)
    n_loc = 3 * BLOCK
    o = (jnp.einsum("bhgnqk,bnkhd->bnqhgd", p[..., :n_loc], vb)
         + jnp.einsum("bhgnqc,bchd->bnqhgd", p[..., n_loc:n_loc + C], vc))
    return o.reshape(B, S, ATTN_Q_W)


def context_attention(q, kc, vc, sink):
    B, C = q.shape[:2]
    qg = q.reshape(B, C, ATTN_KV_HEADS, ATTN_GROUP, HEAD_DIM)
    s = jnp.einsum("bqhgd,bkhd->bhgqk", qg, kc).astype(jnp.float32) * (HEAD_DIM ** -0.5)
    sink_l = jnp.broadcast_to(sink.astype(jnp.float32).reshape(1, ATTN_KV_HEADS, ATTN_GROUP, 1, 1),
                              s.shape[:-1] + (1,))
    p = jax.nn.softmax(jnp.concatenate([s, sink_l], axis=-1), axis=-1)[..., :C].astype(vc.dtype)
    return jnp.einsum("bhgqk,bkhd->bqhgd", p, vc).reshape(B, C, ATTN_Q_W)


def mlstm_chunk_states(k, v, logi, logf, state0):
    B, H, T, _ = k.shape
    N = T // MLSTM_CHUNK
    kc = k.reshape(B, H, N, MLSTM_CHUNK, MLSTM_QK_DIM)
    vc = v.reshape(B, H, N, MLSTM_CHUNK, MLSTM_V_DIM)
    b = jnp.cumsum(logf.reshape(B, H, N, MLSTM_CHUNK), axis=-1)
    b_end = b[..., -1]
    w = b_end[..., None] - b + logi.reshape(B, H, N, MLSTM_CHUNK)
    a = jnp.max(w, axis=-1)
    e = jnp.exp(w - a[..., None])
    c_loc = jnp.einsum("bhnl,bhnlk,bhnlv->bhnkv", e, kc, vc)
    n_loc = jnp.einsum("bhnl,bhnlk->bhnk", e, kc)

    def step(carry, xs):
        C, n, m = carry
        cl, nl, bl, al = xs
        m_new = jnp.maximum(bl + m, al)
        f_prev = jnp.exp(bl + m - m_new)
        f_loc = jnp.exp(al - m_new)
        C_new = f_prev[..., None, None] * C + f_loc[..., None, None] * cl
        n_new = f_prev[..., None] * n + f_loc[..., None] * nl
        return (C_new, n_new, m_new), (C, n, m)

    xs = tuple(jnp.moveaxis(t, 2, 0) for t in (c_loc, n_loc, b_end, a))
    final, starts = lax.scan(step, state0, xs)
    starts = tuple(jnp.moveaxis(t, 0, 2) for t in starts)
    return starts, final


def mlstm_chunk_outputs(q, k, v, logi, logf, starts):
    B, H, T, _ = q.shape
    N = T // MLSTM_CHUNK
    L = MLSTM_CHUNK
    qc = q.reshape(B, H, N, L, MLSTM_QK_DIM)
    kc = k.reshape(B, H, N, L, MLSTM_QK_DIM)
    vc = v.reshape(B, H, N, L, MLSTM_V_DIM)
    li = logi.reshape(B, H, N, L)
    b = jnp.cumsum(logf.reshape(B, H, N, L), axis=-1)
    C0, n0, m0 = starts
    order = jnp.arange(L)[:, None] >= jnp.arange(L)[None, :]
    w = jnp.where(order, b[..., :, None] - b[..., None, :] + li[..., None, :], -jnp.inf)
    inter = b + m0[..., None]
    m = jnp.maximum(inter, jnp.max(w, axis=-1))
    s = jnp.einsum("bhntk,bhnsk->bhnts", qc, kc) * jnp.exp(w - m[..., None])
    e_inter = jnp.exp(inter - m)
    num = (jnp.einsum("bhnts,bhnsv->bhntv", s, vc)
           + e_inter[..., None] * jnp.einsum("bhntk,bhnkv->bhntv", qc, C0))
    den = jnp.sum(s, axis=-1) + e_inter * jnp.einsum("bhntk,bhnk->bhnt", qc, n0)
    h = num / jnp.maximum(jnp.abs(den), jnp.exp(-m))[..., None]
    return h.reshape(B, H, T, MLSTM_V_DIM)


def _maybe_flip(t, rev):
    return jnp.flip(t, axis=2) if rev else t


def mlstm_mixer(q, k, v, g, kc, vc, gc, qc, b_gates):
    B = q.shape[0]
    H = MLSTM_HEADS
    scale = MLSTM_QK_DIM ** -0.5

    def heads(t, d):
        return t.astype(jnp.float32).reshape(t.shape[0], t.shape[1], H, d).transpose(0, 2, 1, 3)

    def gates(t):
        z = (t.astype(jnp.float32) + b_gates.astype(jnp.float32)).reshape(t.shape[0], t.shape[1], N_DIRS, 2, H)
        z = z.transpose(2, 3, 0, 4, 1)
        return z[:, 0], jax.nn.log_sigmoid(z[:, 1])

    ql, kl, vl = heads(q, MLSTM_QK_DIM) * scale, heads(k, MLSTM_QK_DIM), heads(v, MLSTM_V_DIM)
    li, lf = gates(g)
    kcc, vcc = heads(kc, MLSTM_QK_DIM), heads(vc, MLSTM_V_DIM)
    lic, lfc = gates(gc)
    qcc = None if qc is None else heads(qc, MLSTM_QK_DIM) * scale
    state0 = (jnp.zeros((B, H, MLSTM_QK_DIM, MLSTM_V_DIM), jnp.float32),
              jnp.zeros((B, H, MLSTM_QK_DIM), jnp.float32),
              jnp.zeros((B, H), jnp.float32))
    lat_out, ctx_out = [], []
    for d in range(N_DIRS):
        rev = d == 1
        c_starts, c_final = mlstm_chunk_states(_maybe_flip(kcc, rev), _maybe_flip(vcc, rev),
                                               _maybe_flip(lic[d], rev), _maybe_flip(lfc[d], rev), state0)
        l_starts, _ = mlstm_chunk_states(_maybe_flip(kl, rev), _maybe_flip(vl, rev),
                                         _maybe_flip(li[d], rev), _maybe_flip(lf[d], rev), c_final)
        lat_out.append(_maybe_flip(mlstm_chunk_outputs(
            _maybe_flip(ql, rev), _maybe_flip(kl, rev), _maybe_flip(vl, rev),
            _maybe_flip(li[d], rev), _maybe_flip(lf[d], rev), l_starts), rev))
        if qcc is not None:
            ctx_out.append(_maybe_flip(mlstm_chunk_outputs(
                _maybe_flip(qcc, rev), _maybe_flip(kcc, rev), _maybe_flip(vcc, rev),
                _maybe_flip(lic[d], rev), _maybe_flip(lfc[d], rev), c_starts), rev))
    h_lat = lat_out[0] + lat_out[1]
    h_ctx = (ctx_out[0] + ctx_out[1]) if qcc is not None else None
    return h_lat, h_ctx


def head_norm(h, g):
    h = h * lax.rsqrt(jnp.mean(h * h, axis=-1, keepdims=True) + NORM_EPS)
    B, H, T, dv = h.shape
    return h.transpose(0, 2, 1, 3).reshape(B, T, H * dv) * g.astype(jnp.float32)


def merge_branches(att, mem, g_att, g_mem, w_attn_proj, w_mlstm_proj, w_out):
    y = jax.nn.sigmoid(g_att) * (att @ w_attn_proj) + jax.nn.sigmoid(g_mem) * (mem @ w_mlstm_proj)
    return y @ w_out


def swiglu(h, w_ffn_in, w_ffn_out):
    gt, up = jnp.split(h @ w_ffn_in, 2, axis=-1)
    return (jax.nn.silu(gt) * up) @ w_ffn_out


def trunk_layer(x, xc, mod, mod_c, rows, cols, norm1_g, w_in, b_gates, attn_sink, mlstm_norm_g,
                w_attn_proj, w_mlstm_proj, w_out, norm2_g, w_ffn_in, w_ffn_out, ctx_out):
    B, S, _ = x.shape
    C = xc.shape[1]
    shift1, scale1, gate1, shift2, scale2, gate2 = mod
    h = modulate(rms_norm(x, norm1_g), shift1, scale1)
    hc = modulate(rms_norm(xc, norm1_g), mod_c[0], mod_c[1])
    p = h @ w_in
    pc = hc @ (w_in if ctx_out else w_in[:, :N_KV_SIDE])
    a_k, a_v, m_k, m_v, m_g = _split(p[..., :N_KV_SIDE], KV_SIDE_SIZES)
    a_q, m_q, m_o, g_att, g_mem = _split(p[..., N_KV_SIDE:], Q_SIDE_SIZES)
    ac_k, ac_v, mc_k, mc_v, mc_g = _split(pc[..., :N_KV_SIDE], KV_SIDE_SIZES)
    mc_q = None
    if ctx_out:
        ac_q, mc_q, mc_o, gc_att, gc_mem = _split(pc[..., N_KV_SIDE:], Q_SIDE_SIZES)

    q = rope_2d(a_q.reshape(B, S, ATTN_HEADS, HEAD_DIM), rows, cols)
    k = rope_2d(a_k.reshape(B, S, ATTN_KV_HEADS, HEAD_DIM), rows, cols)
    v = a_v.reshape(B, S, ATTN_KV_HEADS, HEAD_DIM)
    kc = ac_k.reshape(B, C, ATTN_KV_HEADS, HEAD_DIM)
    vc = ac_v.reshape(B, C, ATTN_KV_HEADS, HEAD_DIM)
    att = window_attention(q, k, v, kc, vc, attn_sink)

    h_mem, hc_mem = mlstm_mixer(m_q, m_k, m_v, m_g, mc_k, mc_v, mc_g, mc_q, b_gates)
    mem = (jax.nn.sigmoid(m_o.astype(jnp.float32)) * head_norm(h_mem, mlstm_norm_g)).astype(x.dtype)

    x = x + gate1 * merge_branches(att, mem, g_att, g_mem, w_attn_proj, w_mlstm_proj, w_out)
    x = x + gate2 * swiglu(modulate(rms_norm(x, norm2_g), shift2, scale2), w_ffn_in, w_ffn_out)

    if ctx_out:
        att_c = context_attention(ac_q.reshape(B, C, ATTN_HEADS, HEAD_DIM), kc, vc, attn_sink)
        mem_c = (jax.nn.sigmoid(mc_o.astype(jnp.float32)) * head_norm(hc_mem, mlstm_norm_g)).astype(xc.dtype)
        xc = xc + mod_c[2] * merge_branches(att_c, mem_c, gc_att, gc_mem, w_attn_proj, w_mlstm_proj, w_out)
        xc = xc + mod_c[5] * swiglu(modulate(rms_norm(xc, norm2_g), mod_c[3], mod_c[4]), w_ffn_in, w_ffn_out)
    return x, xc


def setup_inputs(seed: int = 0) -> dict:
    key = jax.random.key(seed)
    ks = jax.random.split(key, 20)
    nrm = jax.random.normal
    D = D_MODEL
    f_base = jnp.stack([jnp.zeros((MLSTM_HEADS,), jnp.float32),
                        jnp.linspace(FGATE_BIAS_LO, FGATE_BIAS_HI, MLSTM_HEADS, dtype=jnp.float32)])
    b_gates = (f_base[None, None] + 0.1 * nrm(ks[8], (DEPTH, N_DIRS, 2, MLSTM_HEADS), jnp.float32)
               ).reshape(DEPTH, N_GATE)
    return {
        "x": nrm(ks[0], (BATCH, SEQ, D), jnp.float32),
        "c": nrm(ks[1], (BATCH, D), jnp.float32),
        "ctx": nrm(ks[2], (BATCH, CTX_LEN, D), jnp.float32),
        "c_ctx": nrm(ks[3], (D,), jnp.float32),
        "w_ada": nrm(ks[4], (DEPTH, D, N_MOD * D), jnp.float32) * D ** -0.5,
        "b_ada": 0.01 * nrm(ks[5], (DEPTH, N_MOD * D), jnp.float32),
        "norm1_g": 1.0 + 0.01 * nrm(ks[6], (DEPTH, D), jnp.float32),
        "w_in": nrm(ks[7], (DEPTH, D, N_IN), jnp.float32) * D ** -0.5,
        "b_gates": b_gates,
        "attn_sink": 0.5 * nrm(ks[9], (DEPTH, ATTN_HEADS), jnp.float32),
        "mlstm_norm_g": 1.0 + 0.01 * nrm(ks[10], (DEPTH, MLSTM_V_W), jnp.float32),
        "w_attn_proj": nrm(ks[11], (DEPTH, ATTN_Q_W, D), jnp.float32) * ATTN_Q_W ** -0.5,
        "w_mlstm_proj": nrm(ks[12], (DEPTH, MLSTM_V_W, D), jnp.float32) * MLSTM_V_W ** -0.5,
        "w_out": nrm(ks[13], (DEPTH, D, D), jnp.float32) * D ** -0.5,
        "norm2_g": 1.0 + 0.01 * nrm(ks[14], (DEPTH, D), jnp.float32),
        "w_ffn_in": nrm(ks[15], (DEPTH, D, 2 * D_FF), jnp.float32) * D ** -0.5,
        "w_ffn_out": nrm(ks[16], (DEPTH, D_FF, D), jnp.float32) * D_FF ** -0.5,
        "final_norm_g": 1.0 + 0.01 * nrm(ks[17], (D,), jnp.float32),
    }


def reference(x, c, ctx, c_ctx, w_ada, b_ada, norm1_g, w_in, b_gates, attn_sink, mlstm_norm_g,
              w_attn_proj, w_mlstm_proj, w_out, norm2_g, w_ffn_in, w_ffn_out, final_norm_g):
    S = x.shape[1]
    rows_n = S // GRID_W
    rows, cols = jnp.meshgrid(jnp.arange(rows_n), jnp.arange(GRID_W), indexing="ij")
    rows = rows.reshape(-1)
    cols = cols.reshape(-1)
    xc = ctx
    for l in range(DEPTH):
        ctx_out = l < DEPTH - 1
        mod = adaln_params(c[:, None, :], w_ada[l], b_ada[l], N_MOD)
        mod_c = adaln_params(c_ctx, w_ada[l], b_ada[l], N_MOD if ctx_out else 2)
        x, xc = trunk_layer(x, xc, mod, mod_c, rows, cols, norm1_g[l], w_in[l], b_gates[l], attn_sink[l],
                            mlstm_norm_g[l], w_attn_proj[l], w_mlstm_proj[l], w_out[l], norm2_g[l],
                            w_ffn_in[l], w_ffn_out[l], ctx_out)
    return rms_norm(x, final_norm_g)
```

```python
import os
import numpy as np
import concourse.bass as bass
import concourse.mybir as mybir
from concourse.bass_utils import run_bass_kernel_spmd

F32 = mybir.dt.float32
BF16 = mybir.dt.bfloat16
AF = mybir.ActivationFunctionType
ALU = mybir.AluOpType

D = 2048
KC = 16
SEQ = 8192
TOWN = 4096
NTO = 32
CTXL = 256
DFF = 5632
EPS = 1e-6
C_AK, C_AV, C_MK, C_MV, C_MG, C_AQ, C_MQ, C_MO, C_GA, C_GM = 0, 512, 1024, 2048, 4096, 4128, 6176, 7200, 9248, 11296
NIN = 13344
NS = 20
BIGW = 47 * 1024
ENG = ['pe', 'act', 'dve', 'pool', 'sp']
PHASES = os.environ.get("MK_PHASES", "APTMGF")


class Op:
    __slots__ = ('eng', 'fn', 'dma', 'needed', 'val', 'deps', 'dsem', 'dval', 'grp')


class Prog:
    def __init__(self):
        self.ops = {e: [] for e in ENG}
        self.lastw = {}
        self.rd = {}
        self.fence_deps = None
        self.fenced = set()
        self.dmas = []
        self.lastc = {}

    def op(self, eng, fn, r=(), w=(), dma=False, grp=None):
        o = Op()
        o.eng = eng; o.fn = fn; o.dma = dma; o.needed = False; o.val = None; o.grp = grp or eng
        deps = []
        for k in r:
            lw = self.lastw.get(k)
            if lw is not None:
                deps.append(lw)
        for k in w:
            lw = self.lastw.get(k)
            if lw is not None:
                deps.append(lw)
            rr = self.rd.get(k)
            if rr:
                deps.extend(rr[0].values()); deps.extend(rr[1])
        if self.fence_deps is not None and eng not in self.fenced:
            deps.extend(self.fence_deps); self.fenced.add(eng)
        for k in r:
            rr = self.rd.setdefault(k, ({}, []))
            if dma:
                rr[1].append(o)
            else:
                rr[0][eng] = o
        for k in w:
            self.lastw[k] = o
            self.rd[k] = ({}, [])
        o.deps = [d for d in deps if d is not o]
        for d in o.deps:
            d.needed = True
        self.ops[eng].append(o)
        if dma:
            if grp != 'cv':
                self.dmas.append(o)
        else:
            self.lastc[eng] = o
        return o

    def fence(self):
        f = list(self.lastc.values()) + self.dmas
        self.fence_deps = f
        self.fenced = set()
        self.dmas = []
        self.lastw = {k: v for k, v in self.lastw.items() if isinstance(k, tuple) and k[0] == 'wq'}
        self.rd = {k: v for k, v in self.rd.items() if isinstance(k, tuple) and k[0] == 'wq'}

    def pe(self, fn, r=(), w=()): return self.op('pe', fn, r, w)
    def act(self, fn, r=(), w=()): return self.op('act', fn, r, w)
    def dve(self, fn, r=(), w=()): return self.op('dve', fn, r, w)
    def pool(self, fn, r=(), w=()): return self.op('pool', fn, r, w)

    def dma(self, q, out, in_, r=(), w=(), grp=None):
        return self.op(q, lambda e: e.dma_start(out=out, in_=in_), r, w, dma=True, grp=grp)

    def resolve(self, sems, dsems):
        self.sems = sems
        self.final = {}
        for e in ENG:
            c = 0; ndg = {}
            for o in self.ops[e]:
                if o.dma:
                    nd = ndg.get(o.grp, 0); ndg[o.grp] = nd + 1
                    pool_ = dsems[o.grp]
                    o.dsem = pool_[nd % len(pool_)]; o.dval = 16 * (nd // len(pool_) + 1)
                    self.final[id(o.dsem)] = (o.dsem, o.dval)
                elif o.needed:
                    c += 1; o.val = c

    def emit(self, e, eng, final_wait=False):
        waited = {}

        def wait(sem, val):
            if waited.get(id(sem), 0) >= val:
                return
            eng.wait_ge(sem, val); waited[id(sem)] = val
        for o in self.ops[e]:
            for d in o.deps:
                if d.dma:
                    wait(d.dsem, d.dval)
                else:
                    if d.eng == e and e == 'pe':
                        continue
                    wait(self.sems[d.eng], d.val)
            if o.dma and o.dval > 16:
                wait(o.dsem, o.dval - 16)
            ins = o.fn(eng)
            if o.dma:
                ins.then_inc(o.dsem, 16)
            elif o.needed:
                ins.then_inc(self.sems[e], 1)
        if final_wait:
            for sem, val in self.final.values():
                wait(sem, val)


def MM(out, lhsT, rhs, start=True, stop=True):
    return lambda e: e.matmul(out, lhsT, rhs, start=start, stop=stop)


def TRP(out, in_, ident):
    return lambda e: e.transpose(out, in_, ident)


def ACT(out, in_, func, **kw):
    return lambda e: e.activation(out=out, in_=in_, func=func, **kw)


def TT(out, a, b, op):
    return lambda e: e.tensor_tensor(out=out, in0=a, in1=b, op=op)


def STT(out, a, s, b, op0, op1):
    return lambda e: e.scalar_tensor_tensor(out=out, in0=a, scalar=s, in1=b, op0=op0, op1=op1)


def TS(out, a, s1, s2, op0, op1=None):
    if op1 is None:
        return lambda e: e.tensor_scalar(out=out, in0=a, scalar1=s1, scalar2=None, op0=op0)
    return lambda e: e.tensor_scalar(out=out, in0=a, scalar1=s1, scalar2=s2, op0=op0, op1=op1)


def CP(out, in_):
    return lambda e: e.tensor_copy(out=out, in_=in_)


def RCP(out, in_):
    return lambda e: e.reciprocal(out=out, in_=in_)


def MEMSET(ap, v):
    return lambda e: e.memset(ap, v)


class Carve:
    def __init__(self, big):
        self.big = big; self.off = 0

    def f32(self, n, inner=None):
        ap = self.big[:, self.off:self.off + n]; self.off += n
        assert self.off <= BIGW, self.off
        if inner:
            ap = ap.rearrange("p (a b) -> p a b", b=inner)
        return ap

    def bf(self, n, inner=None):
        w = (n + 1) // 2
        ap = self.big[:, self.off:self.off + w].bitcast(BF16); self.off += w
        assert self.off <= BIGW, self.off
        if inner:
            ap = ap.rearrange("p (a b) -> p a b", b=inner)
        return ap


class Rot:
    def __init__(self, items, name):
        self.items = items; self.name = name; self.i = 0

    def next(self):
        k = self.i % len(self.items); self.i += 1
        return self.items[k], (self.name, k)


def build(dbg=()):
    nc = bass.Bass("TRN2", target_bir_lowering=False)

    def din(name, shape, dt=F32):
        return nc.dram_tensor(name, list(shape), dt, kind="ExternalInput").ap()

    def dscr(name, shape, dt):
        kind = "ExternalOutput" if name in dbg else "Internal"
        return nc.dram_tensor(name, list(shape), dt, kind=kind).ap()

    xv = din("xv", [SEQ, D]); ctxv = din("ctxv", [CTXL, D])
    crep = din("crep", [128, D]); cctxrep = din("cctxrep", [128, D])
    w_ada = din("w_ada", [D, 6 * D]); bada_rep = din("bada_rep", [128, 6 * D])
    g1rep = din("g1rep", [128, D]); g2rep = din("g2rep", [128, D]); gfrep = din("gfrep", [128, D])
    gmrep = din("gmrep", [128, D])
    w_in = din("w_in", [D, NIN]); w_g = din("w_g", [D, 32]); bg_rep = din("bg_rep", [128, 32])
    sinkrep = din("sinkrep", [128, D])
    w_ap = din("w_ap", [D, D]); w_mp = din("w_mp", [D, D]); w_o = din("w_o", [D, D])
    w_fi = din("w_fi", [D, 2 * DFF]); w_fo = din("w_fo", [DFF, D])
    cident = din("cident", [128, 128]); cpm = din("cpm", [128, 128])
    ctriA = din("ctriA", [128, 512]); ctriB = din("ctriB", [128, 512]); cones = din("cones", [128, 128])
    cnegA = din("cnegA", [128, 512]); cnegB = din("cnegB", [128, 512])
    cosT = din("cosT", [128, TOWN + 128]); sinT = din("sinT", [128, TOWN + 128])
    out = nc.dram_tensor("out", [TOWN, D], F32, kind="ExternalOutput").ap()

    q_in = dscr("q_in", [D, NIN], BF16); q_g = dscr("q_g", [D, 32], BF16)
    q_ap = dscr("q_ap", [D, D], BF16); q_mp = dscr("q_mp", [D, D], BF16); q_o = dscr("q_o", [D, D], BF16)
    q_fi = dscr("q_fi", [D, 2 * DFF], BF16); q_fo = dscr("q_fo", [DFF, D], BF16)
    modsc = dscr("modsc", [8, 128, D], F32)
    aqT_d = dscr("aqT_d", [16, 128, TOWN], BF16)
    akT_d = dscr("akT_d", [4, 128, TOWN + 128 + CTXL], BF16)
    av_d = dscr("av_d", [35, 128, 512], BF16)
    mqT_d = dscr("mqT_d", [8, 128, TOWN], BF16); mkT_d = dscr("mkT_d", [8, 128, TOWN], BF16)
    mk_d = dscr("mk_d", [66, 128, 1024], BF16); mv_d = dscr("mv_d", [66, 128, 2048], BF16)
    gs_d = dscr("gs_d", [66, 128, 32], F32)
    mo_d = dscr("mo_d", [32, 128, D], BF16)
    sgaT_d = dscr("sgaT_d", [16, 128, TOWN], BF16); sgmT_d = dscr("sgmT_d", [16, 128, TOWN], BF16)
    attT_d = dscr("attT_d", [16, 128, TOWN], BF16); memT_d = dscr("memT_d", [16, 128, TOWN], BF16)
    stB_d = dscr("stB_d", [32, 128, 8 * 257], BF16)
    x2_d = dscr("x2_d", [32, 128, D], F32)
    h2T_d = dscr("h2T_d", [16, 128, TOWN], BF16)

    P = Prog()
    from contextlib import ExitStack
    with ExitStack() as es:
        bigt = es.enter_context(nc.sbuf_tensor("big", [128, BIGW], F32))
        cst = es.enter_context(nc.sbuf_tensor("cst", [128, 2048], F32))
        pst = [es.enter_context(nc.psum_tensor("ps%d" % i, [128, 512], F32)) for i in range(8)]
        sems = {e: es.enter_context(nc.semaphore("s_" + e)) for e in ENG}
        dsems = {e: [es.enter_context(nc.semaphore("d_%s%d" % (e, i))) for i in range(NS)] for e in ('sp', 'pool')}
        dsems['cv'] = [es.enter_context(nc.semaphore("d_cv%d" % i)) for i in range(8)]
        big = bigt[:, :]
        ps = [p[:, :] for p in pst]
        psb = [p[:, :].bitcast(BF16) for p in pst]
        cc = cst[:, :]
        ident = cc[:, 0:64].bitcast(BF16)
        pm_bf = cc[:, 64:128].bitcast(BF16)
        ones_bf = cc[:, 128:192].bitcast(BF16)
        triA4 = cc[:, 192:448].bitcast(BF16)
        triB4 = cc[:, 448:704].bitcast(BF16)
        triA_f = cc[:, 704:832]; triB_f = cc[:, 832:960]; ones_f = cc[:, 960:1088]
        negA4 = cc[:, 1088:1344].bitcast(BF16); negB4 = cc[:, 1344:1600].bitcast(BF16)
        for dst, src in ((ident, cident), (pm_bf, cpm), (ones_bf, cones), (triA4, ctriA), (triB4, ctriB), (negA4, cnegA), (negB4, cnegB)):
            P.dma('pool', dst, src, w=['const'])
        for dst, src in ((triA_f, ctriA[:, 0:128]), (triB_f, ctriB[:, 0:128]), (ones_f, cones)):
            P.dma('sp', dst, src, w=['const'])
        P.fence()
        conv_q = []
        wqkeys = {}
        for (nm, qd, wd, rows) in (('g', q_g, w_g, D), ('in', q_in, w_in, D), ('ap', q_ap, w_ap, D), ('mp', q_mp, w_mp, D),
                                   ('o', q_o, w_o, D), ('fi', q_fi, w_fi, D), ('fo', q_fo, w_fo, DFF)):
            ks = []
            for r0 in range(0, rows, 256):
                ks.append(('wq', nm, r0))
                conv_q.append((qd[r0:r0 + 256, :], wd[r0:r0 + 256, :], ('wq', nm, r0)))
            wqkeys[nm] = ks

        def conv_issue(n):
            for _ in range(n):
                if conv_q:
                    o_, i_, k_ = conv_q.pop(0)
                    P.dma('pool', o_, i_, w=[k_], grp='cv')
        conv_issue(16)

        def wview(q):
            return q.rearrange("(kc p) n -> p kc n", p=128)

        class WStream:
            def __init__(self, WB, blocks, q='pool', rk=(), conv_per=0):
                self.WB = WB; self.blocks = blocks; self.issued = 0; self.q = q; self.rk = list(rk); self.conv_per = conv_per

            def _issue(self):
                i = self.issued
                v, k0, nk, c0, ncol = self.blocks[i]
                sl = i % len(self.WB)
                P.dma(self.q, self.WB[sl][:, 0:nk, 0:ncol], v[:, k0:k0 + nk, c0:c0 + ncol], r=self.rk, w=[('WB', sl)])
                self.issued += 1
                conv_issue(self.conv_per)

            def get(self, i, hold=1):
                while self.issued < min(len(self.blocks), i - hold + 1 + len(self.WB)):
                    self._issue()
                sl = i % len(self.WB)
                return self.WB[sl], ('WB', sl)

        if 'A' in PHASES:
            cv = Carve(big)
            cf = cv.f32(D)
            lhs = [cv.bf(D), cv.bf(D)]
            WB = [cv.bf(16 * 512, 512) for _ in range(3)]
            brep = Rot([cv.f32(512) for _ in range(2)], 'brep')
            grep = Rot([cv.f32(512) for _ in range(2)], 'grep')
            stg = Rot([cv.f32(512) for _ in range(3)], 'stgA')
            PS = Rot(ps[0:4], 'ps')
            for which, src in enumerate((crep, cctxrep)):
                P.dma('sp', cf, src, w=['cf'])
                P.act(ACT(lhs[which], cf, AF.Silu), r=['cf'], w=[('lhs', which)])
            vada = wview(w_ada)
            ws = WStream(WB, [(vada, 0, 16, n * 512, 512) for n in range(24)])
            slotmap = {(0, 0): 1, (0, 1): 0, (0, 2): 2, (0, 3): 4, (0, 4): 3, (0, 5): 5, (1, 0): 7, (1, 1): 6}
            for n in range(24):
                wb, wk = ws.get(n)
                v, cb = n // 4, n % 4
                for which in ((0, 1) if n < 8 else (0,)):
                    pt, pk = PS.next()
                    for kc in range(16):
                        P.pe(MM(pt, lhs[which][:, kc * 128:(kc + 1) * 128], wb[:, kc, :], kc == 0, kc == 15),
                             r=[wk, ('lhs', which)], w=[pk])
                    br, bk = brep.next()
                    P.dma('sp', br, bada_rep[:, n * 512:(n + 1) * 512], w=[bk])
                    st, sk = stg.next()
                    P.dve(TT(st, pt, br, ALU.add), r=[pk, bk], w=[sk])
                    if v in (1, 4):
                        gr, gk = grep.next()
                        P.dma('sp', gr, (g1rep if v == 1 else g2rep)[:, cb * 512:(cb + 1) * 512], w=[gk])
                        P.dve(STT(st, st, 1.0, gr, ALU.add, ALU.mult), r=[sk, gk], w=[sk])
                    P.dma('sp', modsc[slotmap[(which, v)], :, cb * 512:(cb + 1) * 512], st, r=[sk], w=['modsc'])
            P.fence()

        if 'P' in PHASES:
            cv = Carve(big)
            TS_ = 1024
            Am = cv.f32(D); Bm = cv.f32(D)
            xt = Rot([cv.f32(D) for _ in range(2)], 'xt')
            tmp = cv.f32(D)
            hb = Rot([cv.bf(D) for _ in range(2)], 'hb')
            junk = cv.bf(D)
            ssq = cv.f32(80); rstd = cv.f32(80)
            hTs = [cv.bf(16 * TS_, TS_) for _ in range(2)]
            WB = [cv.bf(16 * 512, 512) for _ in range(3)]
            cosS = cv.f32(TS_); sinS = cv.f32(TS_)
            stg = Rot([cv.bf(512) for _ in range(3)], 'stg')
            qb = Rot([cv.bf(512) for _ in range(2)], 'qb')
            t1 = Rot([cv.f32(512) for _ in range(2)], 't1')
            t2 = Rot([cv.f32(512) for _ in range(2)], 't2')
            wg_bf = cv.bf(16 * 32, 32)
            bg = cv.f32(32)
            gz = Rot([cv.f32(32) for _ in range(2)], 'gz')
            ge = Rot([cv.f32(16) for _ in range(2)], 'ge')
            PS = Rot(ps[0:6], 'ps')
            PT = Rot(psb[6:8], 'pt')
            P.dma('sp', wg_bf, wview(q_g), r=wqkeys['g'], w=['wg'])
            P.dma('sp', bg, bg_rep, w=['bg'])
            vin = wview(q_in)
            tcount = [0]
            evac_rr = [0]

            def n1_part1(xsrc):
                c = tcount[0] % 80; tcount[0] += 1
                x_, xk = xt.next()
                P.dma('sp', x_, xsrc, w=[xk])
                P.act(ACT(junk, x_, AF.Square, accum_out=ssq[:, c:c + 1]), r=[xk], w=['junk', ('ss', c)])
                P.dve(TS(rstd[:, c:c + 1], ssq[:, c:c + 1], 1.0 / D, EPS, ALU.mult, ALU.add), r=[('ss', c)], w=[('rs', c)])
                P.act(ACT(rstd[:, c:c + 1], rstd[:, c:c + 1], AF.Sqrt), r=[('rs', c)], w=[('rs', c)])
                P.dve(RCP(rstd[:, c:c + 1], rstd[:, c:c + 1]), r=[('rs', c)], w=[('rs', c)])
                P.dve(STT(tmp, x_, rstd[:, c:c + 1], Am, ALU.mult, ALU.mult), r=[xk, ('rs', c), 'AB'], w=['tmp'])
                h_, hk = hb.next()
                P.dve(TT(h_, tmp, Bm, ALU.add), r=['tmp', 'AB'], w=[hk])
                return h_, hk

            def n1_part2(hp_, i, h_, hk):
                hTd = hTs[hp_]
                for half in range(2):
                    pt, pk = PT.next()
                    for j in range(8):
                        kc = half * 8 + j
                        P.pe(TRP(pt[:, j * 128:(j + 1) * 128], h_[:, kc * 128:(kc + 1) * 128], ident), r=[hk], w=[pk])
                    dst = hTd[:, half * 8:half * 8 + 8, i * 128:(i + 1) * 128]
                    src = pt.rearrange("p (a b) -> p a b", b=128)
                    if half == 0:
                        P.act(ACT(dst, src, AF.Copy), r=[pk], w=[('hT', hp_, i)])
                    else:
                        P.dve(CP(dst, src), r=[pk], w=[('hT', hp_, i)])

            def evac_store(pt, pk, n, dst, func=None, scale=None):
                s_, sk = stg.next()
                if func is not None:
                    kw = {} if scale is None else {'scale': scale}
                    P.act(ACT(s_[:, :n], pt[:, :n], func, **kw), r=[pk], w=[sk])
                else:
                    evac_rr[0] += 1
                    if evac_rr[0] % 2:
                        P.act(ACT(s_[:, :n], pt[:, :n], AF.Copy), r=[pk], w=[sk])
                    else:
                        P.dve(CP(s_[:, :n], pt[:, :n]), r=[pk], w=[sk])
                P.dma('sp', dst, s_[:, :n], r=[sk], w=['dram'])

            def rope_store(pt, pk, n, toff, dst):
                q_, qk = qb.next()
                P.act(ACT(q_[:, :n], pt[:, :n], AF.Copy), r=[pk], w=[qk])
                p2, p2k = PS.next()
                P.pe(MM(p2[:, :n], pm_bf, q_[:, :n]), r=[qk], w=[p2k])
                a_, ak = t1.next(); b_, bk = t2.next()
                P.dve(TT(a_[:, :n], q_[:, :n], cosS[:, toff:toff + n], ALU.mult), r=[qk, 'cs'], w=[ak])
                P.dve(TT(b_[:, :n], p2[:, :n], sinS[:, toff:toff + n], ALU.mult), r=[p2k, 'cs'], w=[bk])
                s_, sk = stg.next()
                P.dve(TT(s_[:, :n], a_[:, :n], b_[:, :n], ALU.add), r=[ak, bk], w=[sk])
                P.dma('sp', dst, s_[:, :n], r=[sk], w=['dram'])

            def gates_store(pt, pk, dst):
                z_, zk = gz.next(); e_, ek = ge.next()
                P.dve(TT(z_, pt[:, 0:32], bg, ALU.add), r=[pk, 'bg'], w=[zk])
                z3 = z_.rearrange("p (a b) -> p a b", b=16)
                e3 = e_.rearrange("p (a b) -> p a b", b=8)
                P.act(ACT(e3, z3[:, :, 8:16], AF.Exp, scale=-1.0), r=[zk], w=[ek])
                P.act(ACT(e3, e3, AF.Ln, bias=1.0), r=[ek], w=[ek])
                P.dve(TS(z3[:, :, 8:16], e3, -1.0, None, ALU.mult), r=[ek], w=[zk])
                P.dma('sp', dst, z_, r=[zk], w=['dram'])

            def feat_gemm(wb, wk, ncol, spans, hkeys, epi):
                for cs in range(ncol // 128):
                    for si, (t0, nt) in enumerate(spans):
                        pt, pk = PS.next()
                        for kc in range(16):
                            P.pe(MM(pt[:, :nt], wb[:, kc, cs * 128:(cs + 1) * 128], hT[:, kc, t0:t0 + nt], kc == 0, kc == 15),
                                 r=[wk] + hkeys, w=[pk])
                        epi(pt, pk, cs, si, nt)

            def tok_gemm(wb, wk, ncol, tiles, epi):
                for i in tiles:
                    pt, pk = PS.next()
                    for kc in range(16):
                        P.pe(MM(pt[:, :ncol], hT[:, kc, i * 128:(i + 1) * 128], wb[:, kc, 0:ncol], kc == 0, kc == 15),
                             r=[wk, ('hT', hp, i)], w=[pk])
                    epi(pt, pk, i)

            sts = [('ctx', [(0, ctxv[0:128, :]), (1, ctxv[128:256, :])])]
            for s in range(4):
                sts.append(('pre', [(2 + s * 8 + i, xv[(s * 8 + i) * 128:(s * 8 + i + 1) * 128, :]) for i in range(8)]))
            for s in range(4):
                sts.append(('own', [(34 + s * 8 + i, xv[TOWN + (s * 8 + i) * 128:TOWN + (s * 8 + i + 1) * 128, :]) for i in range(8)]))
            if os.environ.get("MK_PSHORT"):
                sts = [sts[0], sts[4], sts[5]]
            prev_kind = None
            for sti, (kind, tl) in enumerate(sts):
                nt_ = len(tl)
                hp = sti % 2
                hT = hTs[hp]
                if sti == 0:
                    P.dma('sp', Am, modsc[6], w=['AB']); P.dma('sp', Bm, modsc[7], w=['AB'])
                    for i, (gt, src) in enumerate(tl):
                        h_, hk = n1_part1(src)
                        n1_part2(hp, i, h_, hk)
                nxt_tl = sts[sti + 1][1] if sti + 1 < len(sts) else []
                la = {'i': 0, 'pend': None}

                def lookahead(la=la, nxt_tl=nxt_tl, hp=hp, sti=sti, kind=kind):
                    if sti + 1 >= len(sts) or la['i'] > len(nxt_tl):
                        return
                    i_ = la['i']
                    if i_ == 0 and kind == 'ctx':
                        P.dma('sp', Am, modsc[0], w=['AB']); P.dma('sp', Bm, modsc[1], w=['AB'])
                    new = None
                    if i_ < len(nxt_tl):
                        new = (i_,) + n1_part1(nxt_tl[i_][1])
                    if la['pend'] is not None:
                        n1_part2(1 - hp, *la['pend'])
                    la['pend'] = new
                    la['i'] = i_ + 1
                hkeys = [('hT', hp, i) for i in range(nt_)]
                tiles = list(range(nt_))
                gts = [gt for gt, _ in tl]
                last_pre = (kind == 'pre' and gts[-1] == 33)
                if kind == 'own':
                    o0 = (gts[0] - 34) * 128
                    P.dma('sp', cosS, cosT[:, 128 + o0:128 + o0 + TS_], w=['cs'])
                    P.dma('sp', sinS, sinT[:, 128 + o0:128 + o0 + TS_], w=['cs'])
                elif last_pre:
                    P.dma('sp', cosS[:, 0:128], cosT[:, 0:128], w=['cs'])
                    P.dma('sp', sinS[:, 0:128], sinT[:, 0:128], w=['cs'])
                blocks = []
                if kind == 'own':
                    spans = [(0, 512), (512, 512)]

                    def mk_feat_epi(dst_d, mode, scale=None):
                        def epi(pt, pk, cs, si, nt, cb=None):
                            pass
                        return epi
                    blocks.append((C_AK, 512, 'akT'))
                    blocks.append((C_AV, 512, 'av'))
                    for j in range(2):
                        blocks.append((C_MK + j * 512, 512, ('mk', j)))
                    for j in range(4):
                        blocks.append((C_MV + j * 512, 512, ('mv', j)))
                    for j in range(4):
                        blocks.append((C_AQ + j * 512, 512, ('aqT', j)))
                    for j in range(2):
                        blocks.append((C_MQ + j * 512, 512, ('mqT', j)))
                    for j in range(4):
                        blocks.append((C_MO + j * 512, 512, ('mo', j)))
                    for j in range(4):
                        blocks.append((C_GA + j * 512, 512, ('sgaT', j)))
                    for j in range(4):
                        blocks.append((C_GM + j * 512, 512, ('sgmT', j)))
                elif kind == 'pre':
                    if last_pre:
                        blocks.append((C_AK, 512, 'akT'))
                        blocks.append((C_AV, 512, 'av'))
                    for j in range(2):
                        blocks.append((C_MK + j * 512, 512, ('mk', j)))
                    for j in range(4):
                        blocks.append((C_MV + j * 512, 512, ('mv', j)))
                else:
                    blocks.append((C_AK, 512, 'akT'))
                    blocks.append((C_AV, 512, 'av'))
                    for j in range(2):
                        blocks.append((C_MK + j * 512, 512, ('mk', j)))
                    for j in range(4):
                        blocks.append((C_MV + j * 512, 512, ('mv', j)))
                ws = WStream(WB, [(vin, 0, 16, c0, ncol) for (c0, ncol, _) in blocks], rk=wqkeys['in'], conv_per=1)
                def gates_epi(pt, pk, i):
                    gates_store(pt, pk, gs_d[gts[i]])
                for i in tiles:
                    pt, pk = PS.next()
                    for kc in range(16):
                        P.pe(MM(pt[:, 0:32], hT[:, kc, i * 128:(i + 1) * 128], wg_bf[:, kc, :], kc == 0, kc == 15),
                             r=['wg', ('hT', hp, i)], w=[pk])
                    gates_epi(pt, pk, i)
                for bi, (c0, ncol, use) in enumerate(blocks):
                    wb, wk = ws.get(bi)
                    lookahead()
                    name = use if isinstance(use, str) else use[0]
                    j = 0 if isinstance(use, str) else use[1]
                    if name == 'akT':
                        if kind == 'own':
                            def epi(pt, pk, cs, si, nt):
                                o0_ = (gts[0] - 34) * 128 + si * 512
                                rope_store(pt, pk, nt, si * 512, akT_d[cs, :, 128 + o0_:128 + o0_ + nt])
                            feat_gemm(wb, wk, 512, spans, hkeys, epi)
                        elif kind == 'pre':
                            def epi(pt, pk, cs, si, nt):
                                rope_store(pt, pk, nt, 0, akT_d[cs, :, 0:128])
                            feat_gemm(wb, wk, 512, [(7 * 128, 128)], [('hT', hp, 7)], epi)
                        else:
                            def epi(pt, pk, cs, si, nt):
                                evac_store(pt, pk, nt, akT_d[cs, :, TOWN + 128:TOWN + 128 + CTXL])
                            feat_gemm(wb, wk, 512, [(0, 256)], hkeys, epi)
                    elif name == 'av':
                        if kind == 'own':
                            def epi(pt, pk, i):
                                evac_store(pt, pk, 512, av_d[1 + gts[i] - 34])
                            tok_gemm(wb, wk, 512, tiles, epi)
                        elif kind == 'pre':
                            def epi(pt, pk, i):
                                evac_store(pt, pk, 512, av_d[0])
                            tok_gemm(wb, wk, 512, [7], epi)
                        else:
                            def epi(pt, pk, i):
                                evac_store(pt, pk, 512, av_d[33 + i])
                            tok_gemm(wb, wk, 512, tiles, epi)
                    elif name == 'mk':
                        def epi(pt, pk, i, j=j):
                            evac_store(pt, pk, 512, mk_d[gts[i], :, j * 512:(j + 1) * 512])
                        tok_gemm(wb, wk, 512, tiles, epi)
                        if kind == 'own':
                            def epi(pt, pk, cs, si, nt, j=j):
                                o0_ = (gts[0] - 34) * 128 + si * 512
                                evac_store(pt, pk, nt, mkT_d[j * 4 + cs, :, o0_:o0_ + nt])
                            feat_gemm(wb, wk, 512, spans, hkeys, epi)
                    elif name == 'mv':
                        def epi(pt, pk, i, j=j):
                            evac_store(pt, pk, 512, mv_d[gts[i], :, j * 512:(j + 1) * 512])
                        tok_gemm(wb, wk, 512, tiles, epi)
                    elif name == 'mo':
                        def epi(pt, pk, i, j=j):
                            evac_store(pt, pk, 512, mo_d[gts[i] - 34, :, j * 512:(j + 1) * 512], func=AF.Sigmoid)
                        tok_gemm(wb, wk, 512, tiles, epi)
                    elif name == 'aqT':
                        def epi(pt, pk, cs, si, nt, j=j):
                            o0_ = (gts[0] - 34) * 128 + si * 512
                            rope_store(pt, pk, nt, si * 512, aqT_d[j * 4 + cs, :, o0_:o0_ + nt])
                        feat_gemm(wb, wk, 512, spans, hkeys, epi)
                    elif name == 'mqT':
                        def epi(pt, pk, cs, si, nt, j=j):
                            o0_ = (gts[0] - 34) * 128 + si * 512
                            evac_store(pt, pk, nt, mqT_d[j * 4 + cs, :, o0_:o0_ + nt], func=AF.Copy, scale=128.0 ** -0.5)
                        feat_gemm(wb, wk, 512, spans, hkeys, epi)
                    elif name in ('sgaT', 'sgmT'):
                        dd = sgaT_d if name == 'sgaT' else sgmT_d
                        def epi(pt, pk, cs, si, nt, j=j, dd=dd):
                            o0_ = (gts[0] - 34) * 128 + si * 512
                            evac_store(pt, pk, nt, dd[j * 4 + cs, :, o0_:o0_ + nt], func=AF.Sigmoid)
                        feat_gemm(wb, wk, 512, spans, hkeys, epi)
                for _ in range(12):
                    lookahead()
            P.fence()


        if 'T' in PHASES:
            cv = Carve(big)
            KT = cv.bf(4 * 4480, 4480)
            V = cv.bf(35 * 512, 512)
            sinkE = cv.f32(D)
            QT = Rot([cv.bf(D) for _ in range(3)], 'QT')
            PTs = Rot([cv.bf(512) for _ in range(12)], 'PTs')
            dn = Rot([cv.f32(512) for _ in range(2)], 'dn')
            ost = Rot([cv.bf(512) for _ in range(2)], 'ost')
            for g in range(4):
                P.dma('sp', KT[:, g, :], akT_d[g], w=['KT'])
            P.dma('sp', V, av_d.rearrange("t p c -> p t c"), w=['V'])
            P.dma('sp', sinkE, sinkrep, w=['sk'])
            P.act(ACT(sinkE, sinkE, AF.Exp), r=['sk'], w=['sk'])
            PSs = Rot(ps[0:4], 'pss'); PSo = Rot(ps[4:6], 'pso'); PSd = Rot(ps[6:8], 'psd')
            NJ = int(os.environ.get("MK_TN", "32"))
            its = [(j, g) for j in range(NJ) for g in range(4)]
            qcur = {}

            def stageA(j, g):
                if g == 0:
                    q_, qk = QT.next()
                    P.dma('sp', q_.rearrange("p (h q) -> p h q", q=128),
                          aqT_d[:, :, j * 128:(j + 1) * 128].rearrange("h d q -> d h q"), w=[qk])
                    qcur[j] = (q_, qk)
                q_, qk = qcur[j]
                kbs = [(j, negB4), (j + 1, None)] + ([(j + 2, negA4)] if j < 31 else []) + [(33, None), (34, None)]
                pts = []
                for (kt, msk) in kbs:
                    s_, sk_ = PSs.next()
                    P.pe(MM(s_, KT[:, g, kt * 128:(kt + 1) * 128], q_[:, g * 512:(g + 1) * 512], True, msk is None), r=['KT', qk], w=[sk_])
                    if msk is not None:
                        P.pe(MM(s_, ident, msk, False, True), r=[], w=[sk_])
                    p_, pk_ = PTs.next()
                    P.act(ACT(p_, s_, AF.Exp, scale=128.0 ** -0.5), r=[sk_], w=[pk_])
                    pts.append((kt, p_, pk_))
                return pts

            def stageB(j, g, pts):
                po, pok = PSo.next(); pd, pdk = PSd.next()
                for idx, (kt, p_, pk_) in enumerate(pts):
                    first = idx == 0; last = idx == len(pts) - 1
                    P.pe(MM(po, V[:, kt, g * 128:(g + 1) * 128], p_, first, last), r=['V', pk_], w=[pok])
                    P.pe(MM(pd, ones_bf, p_, first, last), r=[pk_], w=[pdk])
                d_, dk_ = dn.next()
                P.dve(TT(d_, pd, sinkE[:, g * 512:(g + 1) * 512], ALU.add), r=[pdk, 'sk'], w=[dk_])
                P.dve(RCP(d_, d_), r=[dk_], w=[dk_])
                o_, ok_ = ost.next()
                P.dve(TT(o_, po, d_, ALU.mult), r=[pok, dk_], w=[ok_])
                P.dma('sp', attT_d[4 * g:4 * g + 4, :, j * 128:(j + 1) * 128].rearrange("h d q -> d h q"),
                      o_.rearrange("p (h q) -> p h q", q=128), r=[ok_], w=['dram'])
            prev = stageA(*its[0])
            for ii, (j, g) in enumerate(its):
                nxt = stageA(*its[ii + 1]) if ii + 1 < len(its) else None
                stageB(j, g, prev)
                prev = nxt
            P.fence()

        if 'M' in PHASES:
            cv = Carve(big)
            G = cv.f32(66 * 32, 32)
            lfc = [cv.f32(528), cv.f32(528)]
            bb = [cv.f32(528), cv.f32(528)]
            ee = [cv.f32(528, 8), cv.f32(528, 8)]
            uu = [cv.f32(528).rearrange("p (t h o) -> p t h o", h=8, o=1) for _ in range(2)]
            th = [cv.f32(528, 8), cv.f32(528, 8)]
            dtmp = cv.f32(528)
            Cst = [cv.f32(8 * 257, 257) for _ in range(2)]
            CbfA = cv.bf(8 * 257 + 8)[:, 0:8 * 257].rearrange("p (h c) -> p h c", c=257)
            CbfB = Rot([cv.bf(8 * 257 + 8)[:, 0:8 * 257] for _ in range(2)], 'CbfB')
            mkt = Rot([cv.bf(1024) for _ in range(3)], 'mkt')
            mvt = Rot([cv.bf(2048, 256) for _ in range(3)], 'mvt')
            vEr = [Rot([cv.bf(8 * 257 + 8)[:, 0:8 * 257].rearrange("p (h c) -> p h c", c=257) for _ in range(2)], 'vE%d' % d) for d in range(2)]
            tmpE = Rot([cv.f32(257) for _ in range(3)], 'tmpE')
            qTr = Rot([cv.bf(1024, 128) for _ in range(2)], 'qTr')
            kTr = Rot([cv.bf(1024, 128) for _ in range(2)], 'kTr')
            mot = Rot([cv.bf(2048) for _ in range(2)], 'mot')
            Sb = Rot([cv.bf(128) for _ in range(2)], 'Sb')
            SmAr = Rot([cv.bf(128) for _ in range(2)], 'SmA')
            SmBr = Rot([cv.bf(128) for _ in range(2)], 'SmB')
            hbuf = cv.f32(2048); gmo = cv.f32(2048); gm = cv.f32(2048)
            hA = Rot([cv.f32(256) for _ in range(2)], 'hA')
            junk = cv.bf(256)
            memr = Rot([cv.bf(2048) for _ in range(2)], 'mem')
            memTs = Rot([cv.bf(2048, 128) for _ in range(2)], 'memTs')
            d2r = Rot([cv.f32(2) for _ in range(6)], 'd2')
            ssm = Rot([cv.f32(8) for _ in range(2)], 'ssm')
            P.dma('sp', G, gs_d.rearrange("t p c -> p t c"), w=['G'])
            P.dma('sp', gm, gmrep, w=['gm'])
            for d in range(2):
                P.dve(CP(lfc[d].rearrange("p (t h) -> p t h", h=8), G[:, :, d * 16 + 8:d * 16 + 16]), r=['G'], w=[('lfc', d)])
            PSr = Rot(ps[0:6], 'psm')
            for d in range(2):
                tri = triA_f if d == 0 else triB_f
                for hf in range(2):
                    c0 = hf * 264
                    pb, pbk = PSr.next(); pe2, pek = PSr.next()
                    P.pe(MM(pb[:, :264], tri, lfc[d][:, c0:c0 + 264]), r=[('lfc', d)], w=[pbk])
                    P.pe(MM(pe2[:, :264], ones_f, lfc[d][:, c0:c0 + 264]), r=[('lfc', d)], w=[pek])
                    thv = th[d].rearrange("p t h -> p (t h)")[:, c0:c0 + 264]
                    eev = ee[d].rearrange("p t h -> p (t h)")[:, c0:c0 + 264]
                    uuv = uu[d].rearrange("p t h o -> p (t h o)")[:, c0:c0 + 264]
                    P.act(ACT(thv, pb[:, :264], AF.Exp, scale=-1.0), r=[pbk], w=[('th', d)])
                    P.act(ACT(eev, pe2[:, :264], AF.Exp), r=[pek], w=[('ee', d)])
                    P.dve(TT(dtmp[:, c0:c0 + 264].rearrange("p (t h) -> p t h", h=8), G[:, hf * 33:hf * 33 + 33, d * 16:d * 16 + 8],
                             pb[:, :264].rearrange("p (t h) -> p t h", h=8), ALU.subtract), r=['G', pbk], w=['dtmp'])
                    P.act(ACT(uuv, dtmp[:, c0:c0 + 264], AF.Exp), r=['dtmp'], w=[('uu', d)])
            for d in range(2):
                P.dve(MEMSET(Cst[d].rearrange("p h c -> p (h c)"), 0.0), w=[('C', d, h) for h in range(8)])
            P.dve(MEMSET(CbfA.rearrange("p h c -> p (h c)"), 0.0), w=[('CbfA', h) for h in range(8)])

            def load_kv(n):
                mk_, mkk = mkt.next(); mv_, mvk = mvt.next()
                P.dma('sp', mk_, mk_d[n], w=[mkk])
                P.dma('sp', mv_.rearrange("p h c -> p (h c)"), mv_d[n], w=[mvk])
                return mk_, mkk, mv_, mvk

            def make_vE(d, n, mv_, mvk):
                v_, vk = vEr[d].next()
                P.dve(TT(v_[:, :, 0:256], mv_, uu[d][:, n].to_broadcast([128, 8, 256]), ALU.mult), r=[mvk, ('uu', d)], w=[vk])
                P.act(ACT(v_[:, :, 256:257], uu[d][:, n], AF.Copy), r=[('uu', d)], w=[vk])
                return v_, vk

            def state_update(d, n, h, mk_, mkk, v_, vk, PSc, cbf=None, cbfk=None):
                cl, clk = PSc.next()
                P.pe(MM(cl[:, :257], mk_[:, h * 128:(h + 1) * 128], v_[:, h, :]), r=[mkk, vk], w=[clk])
                t_, tk = tmpE.next()
                e_ap = ee[d][:, n, h:h + 1]
                P.act(ACT(t_, cl[:, :257], AF.Copy, scale=e_ap), r=[clk, ('ee', d)], w=[tk])
                P.dve(STT(Cst[d][:, h, :], Cst[d][:, h, :], e_ap, t_, ALU.mult, ALU.add), r=[tk, ('ee', d)], w=[('C', d, h)])
                if cbf is not None:
                    P.pool(CP(cbf[:, h, :], Cst[d][:, h, :]), r=[('C', d, h)], w=[cbfk(h)])

            full = not os.environ.get("MK_PSHORT")
            preT = list(range(2, 34)) if full else list(range(26, 34))
            ownT = list(range(34, 66)) if full else list(range(34, 42))
            for n in [0, 1] + preT:
                mk_, mkk, mv_, mvk = load_kv(n)
                v_, vk = make_vE(0, n, mv_, mvk)
                last = (n == preT[-1])
                for h in range(8):
                    state_update(0, n, h, mk_, mkk, v_, vk, PSr, CbfA if last else None, (lambda h: ('CbfA', h)))
            for n in [1, 0] + ownT[::-1]:
                mk_, mkk, mv_, mvk = load_kv(n)
                v_, vk = make_vE(1, n, mv_, mvk)
                if n >= 34:
                    cb_, cbk = CbfB.next()
                    P.pool(CP(cb_, Cst[1].rearrange("p h c -> p (h c)")), r=[('C', 1, h) for h in range(8)], w=[cbk])
                    P.dma('sp', stB_d[n - 34], cb_, r=[cbk], w=['dram_st'])
                for h in range(8):
                    state_update(1, n, h, mk_, mkk, v_, vk, PSr)
            P.fence()
            S4 = Rot([ps[0][:, i * 128:(i + 1) * 128] for i in range(4)], 'S4')
            PNA = Rot(ps[1:3], 'pna'); PNB = Rot(ps[3:5], 'pnb'); PCL = Rot(ps[5:6], 'pcl'); PTm = Rot(psb[6:8], 'ptm')
            tstate = {}

            def prologue(n):
                j = n - 34
                mk_, mkk, mv_, mvk = load_kv(n)
                q_, qk = qTr.next(); k_, kk = kTr.next(); mo_, mok = mot.next(); cb_, cbk = CbfB.next()
                P.dma('sp', q_, mqT_d[:, :, j * 128:(j + 1) * 128].rearrange("h d t -> d h t"), w=[qk])
                P.dma('sp', k_, mkT_d[:, :, j * 128:(j + 1) * 128].rearrange("h d t -> d h t"), w=[kk])
                P.dma('sp', mo_, mo_d[j], w=[mok])
                P.dma('sp', cb_, stB_d[j], r=['dram_st'], w=[cbk])
                vA, vAk = make_vE(0, n, mv_, mvk)
                vB, vBk = make_vE(1, n, mv_, mvk)
                ss_, ssk = ssm.next()
                tstate[n] = dict(mk=(mk_, mkk), q=(q_, qk), k=(k_, kk), mo=(mo_, mok), cb=(cb_, cbk), vA=(vA, vAk), vB=(vB, vBk),
                                 ss=(ss_, ssk), sm={})

            def partA(n, h):
                if n not in tstate:
                    prologue(n)
                t_ = tstate[n]
                q_, qk = t_['q']; k_, kk = t_['k']
                s_, sk_ = S4.next()
                P.pe(MM(s_, k_[:, h, :], q_[:, h, :]), r=[kk, qk], w=[sk_])
                sb_, sbk = Sb.next()
                P.act(ACT(sb_, s_, AF.Copy), r=[sk_], w=[sbk])
                sa_, sak = SmAr.next(); sB_, sBk = SmBr.next()
                P.pool(TT(sa_, sb_, triA4[:, 0:128], ALU.mult), r=[sbk], w=[sak])
                P.pool(TT(sB_, sb_, triB4[:, 0:128], ALU.mult), r=[sbk], w=[sBk])
                t_['sm'][h] = (sa_, sak, sB_, sBk)

            def partB(n, h):
                t_ = tstate[n]
                mk_, mkk = t_['mk']; q_, qk = t_['q']; cb_, cbk = t_['cb']; vA, vAk = t_['vA']; vB, vBk = t_['vB']
                ss_, ssk = t_['ss']
                sa_, sak, sB_, sBk = t_['sm'][h]
                cb3 = cb_.rearrange("p (h c) -> p h c", c=257)
                na, nak = PNA.next(); nb_, nbk = PNB.next()
                P.pe(MM(na[:, :257], sa_, vA[:, h, :], True, False), r=[sak, vAk], w=[nak])
                P.pe(MM(na[:, :257], q_[:, h, :], CbfA[:, h, :], False, True), r=[qk, ('CbfA', h)], w=[nak])
                P.pe(MM(nb_[:, :257], sB_, vB[:, h, :], True, False), r=[sBk, vBk], w=[nbk])
                P.pe(MM(nb_[:, :257], q_[:, h, :], cb3[:, h, :], False, True), r=[qk, cbk], w=[nbk])
                d2, d2k = d2r.next()
                P.dve(TS(d2[:, 0:1], na[:, 256:257], -1.0, na[:, 256:257], ALU.mult, ALU.max), r=[nak], w=[d2k])
                P.dve(TT(d2[:, 0:1], d2[:, 0:1], th[0][:, n, h:h + 1], ALU.max), r=[d2k, ('th', 0)], w=[d2k])
                P.dve(TS(d2[:, 1:2], nb_[:, 256:257], -1.0, nb_[:, 256:257], ALU.mult, ALU.max), r=[nbk], w=[d2k])
                P.dve(TT(d2[:, 1:2], d2[:, 1:2], th[1][:, n, h:h + 1], ALU.max), r=[d2k, ('th', 1)], w=[d2k])
                P.dve(RCP(d2, d2), r=[d2k], w=[d2k])
                ha_, hak = hA.next()
                P.act(ACT(ha_, na[:, 0:256], AF.Copy, scale=d2[:, 0:1]), r=[nak, d2k], w=[hak])
                P.dve(STT(hbuf[:, h * 256:(h + 1) * 256], nb_[:, 0:256], d2[:, 1:2], ha_, ALU.mult, ALU.add),
                      r=[nbk, d2k, hak], w=[('hbuf', h)])
                P.act(ACT(junk, hbuf[:, h * 256:(h + 1) * 256], AF.Square, accum_out=ss_[:, h:h + 1]), r=[('hbuf', h)], w=['junkm', ssk])
                state_update(0, n, h, mk_, mkk, vA, vAk, PCL, CbfA, (lambda h: ('CbfA', h)))

            def epilogue(n):
                j = n - 34
                t_ = tstate.pop(n)
                mo_, mok = t_['mo']; ss_, ssk = t_['ss']
                P.pool(TT(gmo, mo_, gm, ALU.mult), r=[mok, 'gm'], w=['gmo'])
                P.dve(TS(ss_, ss_, 1.0 / 256, EPS, ALU.mult, ALU.add), r=[ssk], w=[ssk])
                P.act(ACT(ss_, ss_, AF.Sqrt), r=[ssk], w=[ssk])
                P.dve(RCP(ss_, ss_), r=[ssk], w=[ssk])
                m_, mk2 = memr.next()
                for h in range(8):
                    P.dve(STT(m_[:, h * 256:(h + 1) * 256], hbuf[:, h * 256:(h + 1) * 256], ss_[:, h:h + 1],
                              gmo[:, h * 256:(h + 1) * 256], ALU.mult, ALU.mult), r=[('hbuf', h), ssk, 'gmo'], w=[mk2])
                mt_, mtk = memTs.next()
                for half in range(2):
                    pt, pk = PTm.next()
                    for jj in range(8):
                        kc = half * 8 + jj
                        P.pe(TRP(pt[:, jj * 128:(jj + 1) * 128], m_[:, kc * 128:(kc + 1) * 128], ident), r=[mk2], w=[pk])
                    P.act(ACT(mt_[:, half * 8:half * 8 + 8, :], pt.rearrange("p (a b) -> p a b", b=128), AF.Copy), r=[pk], w=[mtk])
                P.dma('sp', memT_d[:, :, j * 128:(j + 1) * 128].rearrange("c p t -> p c t"), mt_, r=[mtk], w=['dram'])
            seq = [(n, h) for n in ownT for h in range(8)]
            partA(*seq[0])
            for ii, (n, h) in enumerate(seq):
                if ii + 1 < len(seq):
                    partA(*seq[ii + 1])
                partB(n, h)
                if h == 7:
                    epilogue(n)
            P.fence()

        if 'G' in PHASES:
            cv = Carve(big)
            attT = cv.bf(16 * 512, 512); memT = cv.bf(16 * 512, 512)
            x2b = big[:, 0:4 * D].rearrange("p (i c) -> p i c", c=D)
            yT = cv.bf(16 * 512, 512)
            sgr = Rot([cv.bf(512) for _ in range(4)], 'sg')
            tmp = cv.f32(D)
            hb = Rot([cv.bf(D) for _ in range(2)], 'hb')
            junk = cv.bf(D)
            G1 = cv.f32(D); A2 = cv.f32(D); B2 = cv.f32(D)
            WB = [cv.bf(16 * 512, 512) for _ in range(4)]
            h2Ts = Rot([cv.bf(2048, 128) for _ in range(2)], 'h2Ts')
            t1 = Rot([cv.f32(512) for _ in range(2)], 't1')
            t2 = Rot([cv.f32(512) for _ in range(2)], 't2')
            xp = Rot([cv.f32(512) for _ in range(3)], 'xp')
            ssq = Rot([cv.f32(1) for _ in range(4)], 'ssq')
            P.dma('sp', G1, modsc[2], w=['G1']); P.dma('sp', A2, modsc[3], w=['A2']); P.dma('sp', B2, modsc[4], w=['B2'])
            vap = wview(q_ap); vmp = wview(q_mp); vo = wview(q_o)
            PS = Rot(ps[0:6], 'ps'); PT = Rot(psb[6:8], 'pt')
            nst = int(os.environ.get("MK_GN", "8"))
            blocks = []
            for st in range(nst):
                for fg in range(4):
                    blocks.append((vap, 0, 16, fg * 512, 512)); blocks.append((vmp, 0, 16, fg * 512, 512))
                for nb in range(4):
                    blocks.append((vo, 0, 16, nb * 512, 512))
            conv_issue(1000)
            ws = WStream(WB, blocks, rk=wqkeys['ap'] + wqkeys['mp'] + wqkeys['o'])
            for st in range(nst):
                o0 = st * 512
                P.dma('sp', attT, attT_d[:, :, o0:o0 + 512].rearrange("c p t -> p c t"), w=['R1'])
                P.dma('sp', memT, memT_d[:, :, o0:o0 + 512].rearrange("c p t -> p c t"), w=['R1'])
                for fg in range(4):
                    wa, wak = ws.get(st * 12 + 2 * fg, 2); wm, wmk = ws.get(st * 12 + 2 * fg + 1, 2)
                    for cs in range(4):
                        fc = fg * 4 + cs
                        pa, pak = PS.next(); pm2, pmk = PS.next()
                        for kc in range(16):
                            P.pe(MM(pa, wa[:, kc, cs * 128:(cs + 1) * 128], attT[:, kc, :], kc == 0, kc == 15), r=[wak, 'R1'], w=[pak])
                        for kc in range(16):
                            P.pe(MM(pm2, wm[:, kc, cs * 128:(cs + 1) * 128], memT[:, kc, :], kc == 0, kc == 15), r=[wmk, 'R1'], w=[pmk])
                        sa_, sak = sgr.next(); sm_, smk = sgr.next()
                        P.dma('sp', sa_, sgaT_d[fc, :, o0:o0 + 512], w=[sak])
                        P.dma('sp', sm_, sgmT_d[fc, :, o0:o0 + 512], w=[smk])
                        a_, ak = t1.next(); b_, bk = t2.next()
                        P.dve(TT(a_, pa, sa_, ALU.mult), r=[pak, sak], w=[ak])
                        P.dve(TT(b_, pm2, sm_, ALU.mult), r=[pmk, smk], w=[bk])
                        P.dve(TT(yT[:, fc, :], a_, b_, ALU.add), r=[ak, bk], w=[('yT', fc)])
                ykeys = [('yT', fc) for fc in range(16)]
                for nb in range(4):
                    wo, wok = ws.get(st * 12 + 8 + nb)
                    for i in range(4):
                        pt, pk = PS.next()
                        for kc in range(16):
                            P.pe(MM(pt, yT[:, kc, i * 128:(i + 1) * 128], wo[:, kc, :], kc == 0, kc == 15), r=[wok] + ykeys, w=[pk])
                        x_, xk = xp.next()
                        tok0 = TOWN + o0 + i * 128
                        P.dma('sp', x_, xv[tok0:tok0 + 128, nb * 512:(nb + 1) * 512], w=[xk])
                        a_, ak = t1.next()
                        P.dve(TT(a_, pt, G1[:, nb * 512:(nb + 1) * 512], ALU.mult), r=[pk, 'G1'], w=[ak])
                        P.dve(TT(x2b[:, i, nb * 512:(nb + 1) * 512], a_, x_, ALU.add), r=[ak, xk], w=['R1'])
                for i in range(4):
                    gt = st * 4 + i
                    P.dma('sp', x2_d[gt], x2b[:, i, :], r=['R1'], w=['dram'])
                    s1, s1k = ssq.next()
                    P.act(ACT(junk, x2b[:, i, :], AF.Square, accum_out=s1), r=['R1'], w=['junk', s1k])
                    P.dve(TS(s1, s1, 1.0 / D, EPS, ALU.mult, ALU.add), r=[s1k], w=[s1k])
                    P.act(ACT(s1, s1, AF.Sqrt), r=[s1k], w=[s1k])
                    P.dve(RCP(s1, s1), r=[s1k], w=[s1k])
                    P.dve(STT(tmp, x2b[:, i, :], s1, A2, ALU.mult, ALU.mult), r=['R1', s1k, 'A2'], w=['tmp'])
                    h_, hk = hb.next()
                    P.dve(TT(h_, tmp, B2, ALU.add), r=['tmp', 'B2'], w=[hk])
                    ht_, htk = h2Ts.next()
                    for half in range(2):
                        pt, pk = PT.next()
                        for jj in range(8):
                            kc = half * 8 + jj
                            P.pe(TRP(pt[:, jj * 128:(jj + 1) * 128], h_[:, kc * 128:(kc + 1) * 128], ident), r=[hk], w=[pk])
                        P.act(ACT(ht_[:, half * 8:half * 8 + 8, :], pt.rearrange("p (a b) -> p a b", b=128), AF.Copy), r=[pk], w=[htk])
                    P.dma('sp', h2T_d[:, :, gt * 128:(gt + 1) * 128].rearrange("c p t -> p c t"), ht_, r=[htk], w=['dram'])
            P.fence()

        if 'F' in PHASES:
            cv = Carve(big)
            h2T = cv.bf(16 * 512, 512)
            actT = cv.bf(44 * 512, 512)
            WB = [cv.bf(16 * 512, 512) for _ in range(4)]
            x3 = cv.f32(4 * D, D)
            G2 = cv.f32(D); gF = cv.f32(D)
            t1 = Rot([cv.f32(512) for _ in range(2)], 't1')
            junk = cv.bf(D)
            ssq = Rot([cv.f32(1) for _ in range(4)], 'ssq')
            P.dma('sp', G2, modsc[5], w=['G2']); P.dma('sp', gF, gfrep, w=['gF'])
            vfi = wview(q_fi); vfo = wview(q_fo)
            PS = Rot(ps[0:4], 'ps')
            nst = int(os.environ.get("MK_GN", "8"))
            blocks = []
            for st in range(nst):
                for fg in range(11):
                    blocks.append((vfi, 0, 16, fg * 512, 512)); blocks.append((vfi, 0, 16, DFF + fg * 512, 512))
                for nb in range(4):
                    for kg in range(4):
                        blocks.append((vfo, kg * 11, 11, nb * 512, 512))
            conv_issue(1000)
            ws = WStream(WB, blocks, rk=wqkeys['fi'] + wqkeys['fo'])
            for st in range(nst):
                o0 = st * 512
                P.dma('sp', h2T, h2T_d[:, :, o0:o0 + 512].rearrange("c p t -> p c t"), w=['h2T'])
                for i in range(4):
                    P.dma('sp', x3[:, i, :], x2_d[st * 4 + i], w=[('x3', i)])
                for fg in range(11):
                    wg_, wgk = ws.get(st * 38 + 2 * fg, 2); wu_, wuk = ws.get(st * 38 + 2 * fg + 1, 2)
                    for cs in range(4):
                        fc = fg * 4 + cs
                        pg, pgk = PS.next(); pu, puk = PS.next()
                        for kc in range(16):
                            P.pe(MM(pg, wg_[:, kc, cs * 128:(cs + 1) * 128], h2T[:, kc, :], kc == 0, kc == 15), r=[wgk, 'h2T'], w=[pgk])
                        for kc in range(16):
                            P.pe(MM(pu, wu_[:, kc, cs * 128:(cs + 1) * 128], h2T[:, kc, :], kc == 0, kc == 15), r=[wuk, 'h2T'], w=[puk])
                        a_, ak = t1.next()
                        P.act(ACT(a_, pg, AF.Silu), r=[pgk], w=[ak])
                        P.dve(TT(actT[:, fc, :], a_, pu, ALU.mult), r=[ak, puk], w=[('actT', fc)])
                akeys = [('actT', fc) for fc in range(44)]
                for nb in range(4):
                    for kg in range(4):
                        wo, wok = ws.get(st * 38 + 22 + nb * 4 + kg)
                        for i in range(4):
                            for k in range(11):
                                kc = kg * 11 + k
                                P.pe(MM(ps[4 + i], actT[:, kc, i * 128:(i + 1) * 128], wo[:, k, :], kg == 0 and k == 0, kg == 3 and k == 10),
                                     r=[wok] + akeys, w=[('acc', i)])
                    for i in range(4):
                        a_, ak = t1.next()
                        P.dve(TT(a_, ps[4 + i], G2[:, nb * 512:(nb + 1) * 512], ALU.mult), r=[('acc', i), 'G2'], w=[ak])
                        P.dve(TT(x3[:, i, nb * 512:(nb + 1) * 512], a_, x3[:, i, nb * 512:(nb + 1) * 512], ALU.add), r=[ak, ('x3', i)], w=[('x3', i)])
                for i in range(4):
                    gt = st * 4 + i
                    s1, s1k = ssq.next()
                    P.act(ACT(junk, x3[:, i, :], AF.Square, accum_out=s1), r=[('x3', i)], w=['junk', s1k])
                    P.dve(TS(s1, s1, 1.0 / D, EPS, ALU.mult, ALU.add), r=[s1k], w=[s1k])
                    P.act(ACT(s1, s1, AF.Sqrt), r=[s1k], w=[s1k])
                    P.dve(RCP(s1, s1), r=[s1k], w=[s1k])
                    P.dve(STT(x3[:, i, :], x3[:, i, :], s1, gF, ALU.mult, ALU.mult), r=[('x3', i), s1k, 'gF'], w=[('x3', i)])
                    P.dma('sp', out[gt * 128:(gt + 1) * 128, :], x3[:, i, :], r=[('x3', i)], w=['out'])
            P.fence()

        P.resolve(sems, dsems)
        with nc.Block() as block:
            @block.tensor
            def _(e):
                P.emit('pe', e)

            @block.scalar
            def _(e):
                P.emit('act', e)

            @block.vector
            def _(e):
                P.emit('dve', e)

            @block.gpsimd
            def _(e):
                P.emit('pool', e)

            @block.sync
            def _(e):
                P.emit('sp', e, final_wait=True)
    return nc


def _rep(v, n=128):
    return np.ascontiguousarray(np.broadcast_to(np.asarray(v, np.float32).reshape(1, -1), (n, v.size)))


def make_in_maps(x, c, ctx, c_ctx, w_ada, b_ada, norm1_g, w_in, b_gates, attn_sink, mlstm_norm_g,
                 w_attn_proj, w_mlstm_proj, w_out, norm2_g, w_ffn_in, w_ffn_out, final_norm_g):
    f = np.float32
    x = np.asarray(x, f); ctx = np.asarray(ctx, f); c = np.asarray(c, f); c_ctx = np.asarray(c_ctx, f)
    W = dict(w_ada=np.ascontiguousarray(np.asarray(w_ada, f)[0]), w_in=np.ascontiguousarray(np.asarray(w_in, f)[0]),
             w_ap=np.ascontiguousarray(np.asarray(w_attn_proj, f)[0]), w_mp=np.ascontiguousarray(np.asarray(w_mlstm_proj, f)[0]),
             w_o=np.ascontiguousarray(np.asarray(w_out, f)[0]), w_fi=np.ascontiguousarray(np.asarray(w_ffn_in, f)[0]),
             w_fo=np.ascontiguousarray(np.asarray(w_ffn_out, f)[0]))
    shared = dict(W)
    shared['bada_rep'] = _rep(np.asarray(b_ada, f)[0])
    shared['g1rep'] = _rep(np.asarray(norm1_g, f)[0]); shared['g2rep'] = _rep(np.asarray(norm2_g, f)[0])
    shared['gfrep'] = _rep(np.asarray(final_norm_g, f)); shared['gmrep'] = _rep(np.asarray(mlstm_norm_g, f)[0])
    shared['sinkrep'] = _rep(np.repeat(np.asarray(attn_sink, f)[0], 128))
    shared['cident'] = np.eye(128, dtype=f)
    pm = np.zeros((128, 128), f)
    for j in range(128):
        partner = j + 32 if (j % 64) < 32 else j - 32
        pm[partner, j] = 1.0
    shared['cpm'] = pm
    s_ = np.arange(128)
    triA = (s_[:, None] <= s_[None, :]).astype(f); triB = (s_[:, None] >= s_[None, :]).astype(f)
    shared['ctriA'] = np.ascontiguousarray(np.tile(triA, (1, 4))); shared['ctriB'] = np.ascontiguousarray(np.tile(triB, (1, 4)))
    shared['cones'] = np.ones((128, 128), f)
    shared['cnegA'] = np.ascontiguousarray(np.tile(np.where(triA > 0, 0.0, -30000.0).astype(f), (1, 4)))
    shared['cnegB'] = np.ascontiguousarray(np.tile(np.where(triB > 0, 0.0, -30000.0).astype(f), (1, 4)))

    def crepf(v):
        return np.ascontiguousarray(np.broadcast_to(v.reshape(16, 128).T[:, :, None], (128, 16, 128)).reshape(128, D))
    shared['cctxrep'] = crepf(c_ctx)
    wg_full = W['w_in'][:, C_MG:C_MG + 32]
    bgf = np.asarray(b_gates, f)[0]
    inv_freq = (np.float32(10000.0) ** (-np.arange(32, dtype=f) / np.float32(32))).astype(f)
    jj = np.arange(128)
    in_maps = []
    for core in range(8):
        b, half = core // 2, core % 2
        flip = (half == 0)
        m = dict(shared)
        m['xv'] = np.ascontiguousarray(x[b][::-1]) if flip else np.ascontiguousarray(x[b])
        m['ctxv'] = np.ascontiguousarray(ctx[b][::-1]) if flip else np.ascontiguousarray(ctx[b])
        m['crep'] = crepf(c[b])
        dA = 1 if flip else 0
        order = list(range(dA * 16, dA * 16 + 16)) + list(range((1 - dA) * 16, (1 - dA) * 16 + 16))
        m['w_g'] = np.ascontiguousarray(wg_full[:, order]); m['bg_rep'] = _rep(bgf[order])
        tpos = np.arange(TOWN - 128, SEQ)
        torig = (SEQ - 1 - tpos) if flip else tpos
        rows = (torig // 64).astype(f); cols = (torig % 64).astype(f)
        pos = np.where((jj < 64)[:, None], rows[None, :], cols[None, :]).astype(f)
        ang = (pos * inv_freq[jj % 32][:, None]).astype(f)
        sgn = np.where((jj % 64) < 32, -1.0, 1.0).astype(f)[:, None]
        m['cosT'] = np.ascontiguousarray(np.cos(ang).astype(f)); m['sinT'] = np.ascontiguousarray((np.sin(ang) * sgn).astype(f))
        in_maps.append(m)
    return in_maps


def kernel(**inputs):
    in_maps = make_in_maps(**inputs)
    nc = build()
    res = run_bass_kernel_spmd(nc, in_maps, core_ids=list(range(8)))
    outp = np.empty((4, SEQ, D), np.float32)
    for core in range(8):
        b, half = core // 2, core % 2
        o = np.asarray(res.results[core]["out"], np.float32)
        if half == 0:
            outp[b, 0:TOWN] = o[::-1]
        else:
            outp[b, TOWN:SEQ] = o
    return outp
```

```python
import os
import numpy as np
import concourse.bass as bass
import concourse.mybir as mybir
from concourse.bass_utils import run_bass_kernel_spmd

F32 = mybir.dt.float32
BF16 = mybir.dt.bfloat16
AF = mybir.ActivationFunctionType
ALU = mybir.AluOpType

D = 2048
KC = 16
SEQ = 8192
TOWN = 4096
NTO = 32
CTXL = 256
DFF = 5632
EPS = 1e-6
C_AK, C_AV, C_MK, C_MV, C_MG, C_AQ, C_MQ, C_MO, C_GA, C_GM = 0, 512, 1024, 2048, 4096, 4128, 6176, 7200, 9248, 11296
NIN = 13344
NS = 20
BIGW = 47 * 1024
ENG = ['pe', 'act', 'dve', 'pool', 'sp']
PHASES = os.environ.get("MK_PHASES", "APTMGF")


class Op:
    __slots__ = ('eng', 'fn', 'dma', 'needed', 'val', 'deps', 'dsem', 'dval', 'grp')


class Prog:
    def __init__(self):
        self.ops = {e: [] for e in ENG}
        self.lastw = {}
        self.rd = {}
        self.fence_deps = None
        self.fenced = set()
        self.dmas = []
        self.lastc = {}

    def op(self, eng, fn, r=(), w=(), dma=False, grp=None):
        o = Op()
        o.eng = eng; o.fn = fn; o.dma = dma; o.needed = False; o.val = None; o.grp = grp or eng
        deps = []
        for k in r:
            lw = self.lastw.get(k)
            if lw is not None:
                deps.append(lw)
        for k in w:
            lw = self.lastw.get(k)
            if lw is not None:
                deps.append(lw)
            rr = self.rd.get(k)
            if rr:
                deps.extend(rr[0].values()); deps.extend(rr[1])
        if self.fence_deps is not None and eng not in self.fenced:
            deps.extend(self.fence_deps); self.fenced.add(eng)
        for k in r:
            rr = self.rd.setdefault(k, ({}, []))
            if dma:
                rr[1].append(o)
            else:
                rr[0][eng] = o
        for k in w:
            self.lastw[k] = o
            self.rd[k] = ({}, [])
        o.deps = [d for d in deps if d is not o]
        for d in o.deps:
            d.needed = True
        self.ops[eng].append(o)
        if dma:
            if grp != 'cv':
                self.dmas.append(o)
        else:
            self.lastc[eng] = o
        return o

    def fence(self):
        f = list(self.lastc.values()) + self.dmas
        self.fence_deps = f
        self.fenced = set()
        self.dmas = []
        self.lastw = {k: v for k, v in self.lastw.items() if isinstance(k, tuple) and k[0] == 'wq'}
        self.rd = {k: v for k, v in self.rd.items() if isinstance(k, tuple) and k[0] == 'wq'}

    def pe(self, fn, r=(), w=()): return self.op('pe', fn, r, w)
    def act(self, fn, r=(), w=()): return self.op('act', fn, r, w)
    def dve(self, fn, r=(), w=()): return self.op('dve', fn, r, w)
    def pool(self, fn, r=(), w=()): return self.op('pool', fn, r, w)

    def dma(self, q, out, in_, r=(), w=(), grp=None):
        return self.op(q, lambda e: e.dma_start(out=out, in_=in_), r, w, dma=True, grp=grp)

    def resolve(self, sems, dsems):
        self.sems = sems
        self.final = {}
        for e in ENG:
            c = 0; ndg = {}
            for o in self.ops[e]:
                if o.dma:
                    nd = ndg.get(o.grp, 0); ndg[o.grp] = nd + 1
                    pool_ = dsems[o.grp]
                    o.dsem = pool_[nd % len(pool_)]; o.dval = 16 * (nd // len(pool_) + 1)
                    self.final[id(o.dsem)] = (o.dsem, o.dval)
                elif o.needed:
                    c += 1; o.val = c

    def emit(self, e, eng, final_wait=False):
        waited = {}

        def wait(sem, val):
            if waited.get(id(sem), 0) >= val:
                return
            eng.wait_ge(sem, val); waited[id(sem)] = val
        for o in self.ops[e]:
            for d in o.deps:
                if d.dma:
                    wait(d.dsem, d.dval)
                else:
                    if d.eng == e and e == 'pe':
                        continue
                    wait(self.sems[d.eng], d.val)
            if o.dma and o.dval > 16:
                wait(o.dsem, o.dval - 16)
            ins = o.fn(eng)
            if o.dma:
                ins.then_inc(o.dsem, 16)
            elif o.needed:
                ins.then_inc(self.sems[e], 1)
        if final_wait:
            for sem, val in self.final.values():
                wait(sem, val)


def MM(out, lhsT, rhs, start=True, stop=True):
    return lambda e: e.matmul(out, lhsT, rhs, start=start, stop=stop)


def TRP(out, in_, ident):
    return lambda e: e.transpose(out, in_, ident)


def ACT(out, in_, func, **kw):
    return lambda e: e.activation(out=out, in_=in_, func=func, **kw)


def TT(out, a, b, op):
    return lambda e: e.tensor_tensor(out=out, in0=a, in1=b, op=op)


def STT(out, a, s, b, op0, op1):
    return lambda e: e.scalar_tensor_tensor(out=out, in0=a, scalar=s, in1=b, op0=op0, op1=op1)


def TS(out, a, s1, s2, op0, op1=None):
    if op1 is None:
        return lambda e: e.tensor_scalar(out=out, in0=a, scalar1=s1, scalar2=None, op0=op0)
    return lambda e: e.tensor_scalar(out=out, in0=a, scalar1=s1, scalar2=s2, op0=op0, op1=op1)


def CP(out, in_):
    return lambda e: e.tensor_copy(out=out, in_=in_)


def RCP(out, in_):
    return lambda e: e.reciprocal(out=out, in_=in_)


def MEMSET(ap, v):
    return lambda e: e.memset(ap, v)


class Carve:
    def __init__(self, big):
        self.big = big; self.off = 0

    def f32(self, n, inner=None):
        ap = self.big[:, self.off:self.off + n]; self.off += n
        assert self.off <= BIGW, self.off
        if inner:
            ap = ap.rearrange("p (a b) -> p a b", b=inner)
        return ap

    def bf(self, n, inner=None):
        w = (n + 1) // 2
        ap = self.big[:, self.off:self.off + w].bitcast(BF16); self.off += w
        assert self.off <= BIGW, self.off
        if inner:
            ap = ap.rearrange("p (a b) -> p a b", b=inner)
        return ap


class Rot:
    def __init__(self, items, name):
        self.items = items; self.name = name; self.i = 0

    def next(self):
        k = self.i % len(self.items); self.i += 1
        return self.items[k], (self.name, k)


def build(dbg=()):
    nc = bass.Bass("TRN2", target_bir_lowering=False)

    def din(name, shape, dt=F32):
        return nc.dram_tensor(name, list(shape), dt, kind="ExternalInput").ap()

    def dscr(name, shape, dt):
        kind = "ExternalOutput" if name in dbg else "Internal"
        return nc.dram_tensor(name, list(shape), dt, kind=kind).ap()

    xv = din("xv", [SEQ, D]); ctxv = din("ctxv", [CTXL, D])
    crep = din("crep", [128, D]); cctxrep = din("cctxrep", [128, D])
    w_ada = din("w_ada", [D, 6 * D]); bada_rep = din("bada_rep", [128, 6 * D])
    g1rep = din("g1rep", [128, D]); g2rep = din("g2rep", [128, D]); gfrep = din("gfrep", [128, D])
    gmrep = din("gmrep", [128, D])
    w_in = din("w_in", [D, NIN]); w_g = din("w_g", [D, 32]); bg_rep = din("bg_rep", [128, 32])
    sinkrep = din("sinkrep", [128, D])
    w_ap = din("w_ap", [D, D]); w_mp = din("w_mp", [D, D]); w_o = din("w_o", [D, D])
    w_fi = din("w_fi", [D, 2 * DFF]); w_fo = din("w_fo", [DFF, D])
    cident = din("cident", [128, 128]); cpm = din("cpm", [128, 128])
    ctriA = din("ctriA", [128, 512]); ctriB = din("ctriB", [128, 512]); cones = din("cones", [128, 128])
    cnegA = din("cnegA", [128, 512]); cnegB = din("cnegB", [128, 512])
    cosT = din("cosT", [128, TOWN + 128]); sinT = din("sinT", [128, TOWN + 128])
    out = nc.dram_tensor("out", [TOWN, D], F32, kind="ExternalOutput").ap()

    q_in = dscr("q_in", [D, NIN], BF16); q_g = dscr("q_g", [D, 32], BF16)
    q_ap = dscr("q_ap", [D, D], BF16); q_mp = dscr("q_mp", [D, D], BF16); q_o = dscr("q_o", [D, D], BF16)
    q_fi = dscr("q_fi", [D, 2 * DFF], BF16); q_fo = dscr("q_fo", [DFF, D], BF16)
    modsc = dscr("modsc", [8, 128, D], F32)
    aqT_d = dscr("aqT_d", [16, 128, TOWN], BF16)
    akT_d = dscr("akT_d", [4, 128, TOWN + 128 + CTXL], BF16)
    av_d = dscr("av_d", [35, 128, 512], BF16)
    mqT_d = dscr("mqT_d", [8, 128, TOWN], BF16); mkT_d = dscr("mkT_d", [8, 128, TOWN], BF16)
    mk_d = dscr("mk_d", [66, 128, 1024], BF16); mv_d = dscr("mv_d", [66, 128, 2048], BF16)
    gs_d = dscr("gs_d", [66, 128, 32], F32)
    mo_d = dscr("mo_d", [32, 128, D], BF16)
    sgaT_d = dscr("sgaT_d", [16, 128, TOWN], BF16); sgmT_d = dscr("sgmT_d", [16, 128, TOWN], BF16)
    attT_d = dscr("attT_d", [16, 128, TOWN], BF16); memT_d = dscr("memT_d", [16, 128, TOWN], BF16)
    stB_d = dscr("stB_d", [32, 128, 8 * 257], BF16)
    x2_d = dscr("x2_d", [32, 128, D], F32)
    h2T_d = dscr("h2T_d", [16, 128, TOWN], BF16)

    P = Prog()
    from contextlib import ExitStack
    with ExitStack() as es:
        bigt = es.enter_context(nc.sbuf_tensor("big", [128, BIGW], F32))
        cst = es.enter_context(nc.sbuf_tensor("cst", [128, 2048], F32))
        pst = [es.enter_context(nc.psum_tensor("ps%d" % i, [128, 512], F32)) for i in range(8)]
        sems = {e: es.enter_context(nc.semaphore("s_" + e)) for e in ENG}
        dsems = {e: [es.enter_context(nc.semaphore("d_%s%d" % (e, i))) for i in range(NS)] for e in ('sp', 'pool')}
        dsems['cv'] = [es.enter_context(nc.semaphore("d_cv%d" % i)) for i in range(8)]
        big = bigt[:, :]
        ps = [p[:, :] for p in pst]
        psb = [p[:, :].bitcast(BF16) for p in pst]
        cc = cst[:, :]
        ident = cc[:, 0:64].bitcast(BF16)
        pm_bf = cc[:, 64:128].bitcast(BF16)
        ones_bf = cc[:, 128:192].bitcast(BF16)
        triA4 = cc[:, 192:448].bitcast(BF16)
        triB4 = cc[:, 448:704].bitcast(BF16)
        triA_f = cc[:, 704:832]; triB_f = cc[:, 832:960]; ones_f = cc[:, 960:1088]
        negA4 = cc[:, 1088:1344].bitcast(BF16); negB4 = cc[:, 1344:1600].bitcast(BF16)
        for dst, src in ((ident, cident), (pm_bf, cpm), (ones_bf, cones), (triA4, ctriA), (triB4, ctriB), (negA4, cnegA), (negB4, cnegB)):
            P.dma('pool', dst, src, w=['const'])
        for dst, src in ((triA_f, ctriA[:, 0:128]), (triB_f, ctriB[:, 0:128]), (ones_f, cones)):
            P.dma('sp', dst, src, w=['const'])
        P.fence()
        conv_q = []
        wqkeys = {}
        for (nm, qd, wd, rows) in (('g', q_g, w_g, D), ('in', q_in, w_in, D), ('ap', q_ap, w_ap, D), ('mp', q_mp, w_mp, D),
                                   ('o', q_o, w_o, D), ('fi', q_fi, w_fi, D), ('fo', q_fo, w_fo, DFF)):
            ks = []
            for r0 in range(0, rows, 256):
                ks.append(('wq', nm, r0))
                conv_q.append((qd[r0:r0 + 256, :], wd[r0:r0 + 256, :], ('wq', nm, r0)))
            wqkeys[nm] = ks

        def conv_issue(n):
            for _ in range(n):
                if conv_q:
                    o_, i_, k_ = conv_q.pop(0)
                    P.dma('pool', o_, i_, w=[k_], grp='cv')
        conv_issue(16)

        def wview(q):
            return q.rearrange("(kc p) n -> p kc n", p=128)

        class WStream:
            def __init__(self, WB, blocks, q='pool', rk=(), conv_per=0):
                self.WB = WB; self.blocks = blocks; self.issued = 0; self.q = q; self.rk = list(rk); self.conv_per = conv_per

            def _issue(self):
                i = self.issued
                v, k0, nk, c0, ncol = self.blocks[i]
                sl = i % len(self.WB)
                P.dma(self.q, self.WB[sl][:, 0:nk, 0:ncol], v[:, k0:k0 + nk, c0:c0 + ncol], r=self.rk, w=[('WB', sl)])
                self.issued += 1
                conv_issue(self.conv_per)

            def get(self, i, hold=1):
                while self.issued < min(len(self.blocks), i - hold + 1 + len(self.WB)):
                    self._issue()
                sl = i % len(self.WB)
                return self.WB[sl], ('WB', sl)

        if 'A' in PHASES:
            cv = Carve(big)
            cf = cv.f32(D)
            lhs = [cv.bf(D), cv.bf(D)]
            WB = [cv.bf(16 * 512, 512) for _ in range(3)]
            brep = Rot([cv.f32(512) for _ in range(2)], 'brep')
            grep = Rot([cv.f32(512) for _ in range(2)], 'grep')
            stg = Rot([cv.f32(512) for _ in range(3)], 'stgA')
            PS = Rot(ps[0:4], 'ps')
            for which, src in enumerate((crep, cctxrep)):
                P.dma('sp', cf, src, w=['cf'])
                P.act(ACT(lhs[which], cf, AF.Silu), r=['cf'], w=[('lhs', which)])
            vada = wview(w_ada)
            ws = WStream(WB, [(vada, 0, 16, n * 512, 512) for n in range(24)])
            slotmap = {(0, 0): 1, (0, 1): 0, (0, 2): 2, (0, 3): 4, (0, 4): 3, (0, 5): 5, (1, 0): 7, (1, 1): 6}
            for n in range(24):
                wb, wk = ws.get(n)
                v, cb = n // 4, n % 4
                for which in ((0, 1) if n < 8 else (0,)):
                    pt, pk = PS.next()
                    for kc in range(16):
                        P.pe(MM(pt, lhs[which][:, kc * 128:(kc + 1) * 128], wb[:, kc, :], kc == 0, kc == 15),
                             r=[wk, ('lhs', which)], w=[pk])
                    br, bk = brep.next()
                    P.dma('sp', br, bada_rep[:, n * 512:(n + 1) * 512], w=[bk])
                    st, sk = stg.next()
                    P.dve(TT(st, pt, br, ALU.add), r=[pk, bk], w=[sk])
                    if v in (1, 4):
                        gr, gk = grep.next()
                        P.dma('sp', gr, (g1rep if v == 1 else g2rep)[:, cb * 512:(cb + 1) * 512], w=[gk])
                        P.dve(STT(st, st, 1.0, gr, ALU.add, ALU.mult), r=[sk, gk], w=[sk])
                    P.dma('sp', modsc[slotmap[(which, v)], :, cb * 512:(cb + 1) * 512], st, r=[sk], w=['modsc'])
            P.fence()

        if 'P' in PHASES:
            cv = Carve(big)
            TS_ = 1024
            Am = cv.f32(D); Bm = cv.f32(D)
            xt = Rot([cv.f32(D) for _ in range(2)], 'xt')
            tmp = cv.f32(D)
            hb = Rot([cv.bf(D) for _ in range(2)], 'hb')
            junk = cv.bf(D)
            ssq = cv.f32(80); rstd = cv.f32(80)
            hTs = [cv.bf(16 * TS_, TS_) for _ in range(2)]
            WB = [cv.bf(16 * 512, 512) for _ in range(3)]
            cosS = cv.f32(TS_); sinS = cv.f32(TS_)
            stg = Rot([cv.bf(512) for _ in range(3)], 'stg')
            qb = Rot([cv.bf(512) for _ in range(2)], 'qb')
            t1 = Rot([cv.f32(512) for _ in range(2)], 't1')
            t2 = Rot([cv.f32(512) for _ in range(2)], 't2')
            wg_bf = cv.bf(16 * 32, 32)
            bg = cv.f32(32)
            gz = Rot([cv.f32(32) for _ in range(2)], 'gz')
            ge = Rot([cv.f32(16) for _ in range(2)], 'ge')
            PS = Rot(ps[0:6], 'ps')
            PT = Rot(psb[6:8], 'pt')
            P.dma('sp', wg_bf, wview(q_g), r=wqkeys['g'], w=['wg'])
            P.dma('sp', bg, bg_rep, w=['bg'])
            vin = wview(q_in)
            tcount = [0]
            evac_rr = [0]

            def n1_part1(xsrc):
                c = tcount[0] % 80; tcount[0] += 1
                x_, xk = xt.next()
                P.dma('sp', x_, xsrc, w=[xk])
                P.act(ACT(junk, x_, AF.Square, accum_out=ssq[:, c:c + 1]), r=[xk], w=['junk', ('ss', c)])
                P.dve(TS(rstd[:, c:c + 1], ssq[:, c:c + 1], 1.0 / D, EPS, ALU.mult, ALU.add), r=[('ss', c)], w=[('rs', c)])
                P.act(ACT(rstd[:, c:c + 1], rstd[:, c:c + 1], AF.Sqrt), r=[('rs', c)], w=[('rs', c)])
                P.dve(RCP(rstd[:, c:c + 1], rstd[:, c:c + 1]), r=[('rs', c)], w=[('rs', c)])
                P.dve(STT(tmp, x_, rstd[:, c:c + 1], Am, ALU.mult, ALU.mult), r=[xk, ('rs', c), 'AB'], w=['tmp'])
                h_, hk = hb.next()
                P.dve(TT(h_, tmp, Bm, ALU.add), r=['tmp', 'AB'], w=[hk])
                return h_, hk

            def n1_part2(hp_, i, h_, hk):
                hTd = hTs[hp_]
                for half in range(2):
                    pt, pk = PT.next()
                    for j in range(8):
                        kc = half * 8 + j
                        P.pe(TRP(pt[:, j * 128:(j + 1) * 128], h_[:, kc * 128:(kc + 1) * 128], ident), r=[hk], w=[pk])
                    dst = hTd[:, half * 8:half * 8 + 8, i * 128:(i + 1) * 128]
                    src = pt.rearrange("p (a b) -> p a b", b=128)
                    if half == 0:
                        P.act(ACT(dst, src, AF.Copy), r=[pk], w=[('hT', hp_, i)])
                    else:
                        P.dve(CP(dst, src), r=[pk], w=[('hT', hp_, i)])

            def evac_store(pt, pk, n, dst, func=None, scale=None):
                s_, sk = stg.next()
                if func is not None:
                    kw = {} if scale is None else {'scale': scale}
                    P.act(ACT(s_[:, :n], pt[:, :n], func, **kw), r=[pk], w=[sk])
                else:
                    evac_rr[0] += 1
                    if evac_rr[0] % 2:
                        P.act(ACT(s_[:, :n], pt[:, :n], AF.Copy), r=[pk], w=[sk])
                    else:
                        P.dve(CP(s_[:, :n], pt[:, :n]), r=[pk], w=[sk])
                P.dma('sp', dst, s_[:, :n], r=[sk], w=['dram'])

            def rope_store(pt, pk, n, toff, dst):
                q_, qk = qb.next()
                P.act(ACT(q_[:, :n], pt[:, :n], AF.Copy), r=[pk], w=[qk])
                p2, p2k = PS.next()
                P.pe(MM(p2[:, :n], pm_bf, q_[:, :n]), r=[qk], w=[p2k])
                a_, ak = t1.next(); b_, bk = t2.next()
                P.dve(TT(a_[:, :n], q_[:, :n], cosS[:, toff:toff + n], ALU.mult), r=[qk, 'cs'], w=[ak])
                P.dve(TT(b_[:, :n], p2[:, :n], sinS[:, toff:toff + n], ALU.mult), r=[p2k, 'cs'], w=[bk])
                s_, sk = stg.next()
                P.dve(TT(s_[:, :n], a_[:, :n], b_[:, :n], ALU.add), r=[ak, bk], w=[sk])
                P.dma('sp', dst, s_[:, :n], r=[sk], w=['dram'])

            def gates_store(pt, pk, dst):
                z_, zk = gz.next(); e_, ek = ge.next()
                P.dve(TT(z_, pt[:, 0:32], bg, ALU.add), r=[pk, 'bg'], w=[zk])
                z3 = z_.rearrange("p (a b) -> p a b", b=16)
                e3 = e_.rearrange("p (a b) -> p a b", b=8)
                P.act(ACT(e3, z3[:, :, 8:16], AF.Exp, scale=-1.0), r=[zk], w=[ek])
                P.act(ACT(e3, e3, AF.Ln, bias=1.0), r=[ek], w=[ek])
                P.dve(TS(z3[:, :, 8:16], e3, -1.0, None, ALU.mult), r=[ek], w=[zk])
                P.dma('sp', dst, z_, r=[zk], w=['dram'])

            def feat_gemm(wb, wk, ncol, spans, hkeys, epi):
                for cs in range(ncol // 128):
                    for si, (t0, nt) in enumerate(spans):
                        pt, pk = PS.next()
                        for kc in range(16):
                            P.pe(MM(pt[:, :nt], wb[:, kc, cs * 128:(cs + 1) * 128], hT[:, kc, t0:t0 + nt], kc == 0, kc == 15),
                                 r=[wk] + hkeys, w=[pk])
                        epi(pt, pk, cs, si, nt)

            def tok_gemm(wb, wk, ncol, tiles, epi):
                for i in tiles:
                    pt, pk = PS.next()
                    for kc in range(16):
                        P.pe(MM(pt[:, :ncol], hT[:, kc, i * 128:(i + 1) * 128], wb[:, kc, 0:ncol], kc == 0, kc == 15),
                             r=[wk, ('hT', hp, i)], w=[pk])
                    epi(pt, pk, i)

            sts = [('ctx', [(0, ctxv[0:128, :]), (1, ctxv[128:256, :])])]
            for s in range(4):
                sts.append(('pre', [(2 + s * 8 + i, xv[(s * 8 + i) * 128:(s * 8 + i + 1) * 128, :]) for i in range(8)]))
            for s in range(4):
                sts.append(('own', [(34 + s * 8 + i, xv[TOWN + (s * 8 + i) * 128:TOWN + (s * 8 + i + 1) * 128, :]) for i in range(8)]))
            if os.environ.get("MK_PSHORT"):
                sts = [sts[0], sts[4], sts[5]]
            prev_kind = None
            for sti, (kind, tl) in enumerate(sts):
                nt_ = len(tl)
                hp = sti % 2
                hT = hTs[hp]
                if sti == 0:
                    P.dma('sp', Am, modsc[6], w=['AB']); P.dma('sp', Bm, modsc[7], w=['AB'])
                    for i, (gt, src) in enumerate(tl):
                        h_, hk = n1_part1(src)
                        n1_part2(hp, i, h_, hk)
                nxt_tl = sts[sti + 1][1] if sti + 1 < len(sts) else []
                la = {'i': 0, 'pend': None}

                def lookahead(la=la, nxt_tl=nxt_tl, hp=hp, sti=sti, kind=kind):
                    if sti + 1 >= len(sts) or la['i'] > len(nxt_tl):
                        return
                    i_ = la['i']
                    if i_ == 0 and kind == 'ctx':
                        P.dma('sp', Am, modsc[0], w=['AB']); P.dma('sp', Bm, modsc[1], w=['AB'])
                    new = None
                    if i_ < len(nxt_tl):
                        new = (i_,) + n1_part1(nxt_tl[i_][1])
                    if la['pend'] is not None:
                        n1_part2(1 - hp, *la['pend'])
                    la['pend'] = new
                    la['i'] = i_ + 1
                hkeys = [('hT', hp, i) for i in range(nt_)]
                tiles = list(range(nt_))
                gts = [gt for gt, _ in tl]
                last_pre = (kind == 'pre' and gts[-1] == 33)
                if kind == 'own':
                    o0 = (gts[0] - 34) * 128
                    P.dma('sp', cosS, cosT[:, 128 + o0:128 + o0 + TS_], w=['cs'])
                    P.dma('sp', sinS, sinT[:, 128 + o0:128 + o0 + TS_], w=['cs'])
                elif last_pre:
                    P.dma('sp', cosS[:, 0:128], cosT[:, 0:128], w=['cs'])
                    P.dma('sp', sinS[:, 0:128], sinT[:, 0:128], w=['cs'])
                blocks = []
                if kind == 'own':
                    spans = [(0, 512), (512, 512)]

                    def mk_feat_epi(dst_d, mode, scale=None):
                        def epi(pt, pk, cs, si, nt, cb=None):
                            pass
                        return epi
                    blocks.append((C_AK, 512, 'akT'))
                    blocks.append((C_AV, 512, 'av'))
                    for j in range(2):
                        blocks.append((C_MK + j * 512, 512, ('mk', j)))
                    for j in range(4):
                        blocks.append((C_MV + j * 512, 512, ('mv', j)))
                    for j in range(4):
                        blocks.append((C_AQ + j * 512, 512, ('aqT', j)))
                    for j in range(2):
                        blocks.append((C_MQ + j * 512, 512, ('mqT', j)))
                    for j in range(4):
                        blocks.append((C_MO + j * 512, 512, ('mo', j)))
                    for j in range(4):
                        blocks.append((C_GA + j * 512, 512, ('sgaT', j)))
                    for j in range(4):
                        blocks.append((C_GM + j * 512, 512, ('sgmT', j)))
                elif kind == 'pre':
                    if last_pre:
                        blocks.append((C_AK, 512, 'akT'))
                        blocks.append((C_AV, 512, 'av'))
                    for j in range(2):
                        blocks.append((C_MK + j * 512, 512, ('mk', j)))
                    for j in range(4):
                        blocks.append((C_MV + j * 512, 512, ('mv', j)))
                else:
                    blocks.append((C_AK, 512, 'akT'))
                    blocks.append((C_AV, 512, 'av'))
                    for j in range(2):
                        blocks.append((C_MK + j * 512, 512, ('mk', j)))
                    for j in range(4):
                        blocks.append((C_MV + j * 512, 512, ('mv', j)))
                ws = WStream(WB, [(vin, 0, 16, c0, ncol) for (c0, ncol, _) in blocks], rk=wqkeys['in'], conv_per=1)
                def gates_epi(pt, pk, i):
                    gates_store(pt, pk, gs_d[gts[i]])
                for i in tiles:
                    pt, pk = PS.next()
                    for kc in range(16):
                        P.pe(MM(pt[:, 0:32], hT[:, kc, i * 128:(i + 1) * 128], wg_bf[:, kc, :], kc == 0, kc == 15),
                             r=['wg', ('hT', hp, i)], w=[pk])
                    gates_epi(pt, pk, i)
                for bi, (c0, ncol, use) in enumerate(blocks):
                    wb, wk = ws.get(bi)
                    lookahead()
                    name = use if isinstance(use, str) else use[0]
                    j = 0 if isinstance(use, str) else use[1]
                    if name == 'akT':
                        if kind == 'own':
                            def epi(pt, pk, cs, si, nt):
                                o0_ = (gts[0] - 34) * 128 + si * 512
                                rope_store(pt, pk, nt, si * 512, akT_d[cs, :, 128 + o0_:128 + o0_ + nt])
                            feat_gemm(wb, wk, 512, spans, hkeys, epi)
                        elif kind == 'pre':
                            def epi(pt, pk, cs, si, nt):
                                rope_store(pt, pk, nt, 0, akT_d[cs, :, 0:128])
                            feat_gemm(wb, wk, 512, [(7 * 128, 128)], [('hT', hp, 7)], epi)
                        else:
                            def epi(pt, pk, cs, si, nt):
                                evac_store(pt, pk, nt, akT_d[cs, :, TOWN + 128:TOWN + 128 + CTXL])
                            feat_gemm(wb, wk, 512, [(0, 256)], hkeys, epi)
                    elif name == 'av':
                        if kind == 'own':
                            def epi(pt, pk, i):
                                evac_store(pt, pk, 512, av_d[1 + gts[i] - 34])
                            tok_gemm(wb, wk, 512, tiles, epi)
                        elif kind == 'pre':
                            def epi(pt, pk, i):
                                evac_store(pt, pk, 512, av_d[0])
                            tok_gemm(wb, wk, 512, [7], epi)
                        else:
                            def epi(pt, pk, i):
                                evac_store(pt, pk, 512, av_d[33 + i])
                            tok_gemm(wb, wk, 512, tiles, epi)
                    elif name == 'mk':
                        def epi(pt, pk, i, j=j):
                            evac_store(pt, pk, 512, mk_d[gts[i], :, j * 512:(j + 1) * 512])
                        tok_gemm(wb, wk, 512, tiles, epi)
                        if kind == 'own':
                            def epi(pt, pk, cs, si, nt, j=j):
                                o0_ = (gts[0] - 34) * 128 + si * 512
                                evac_store(pt, pk, nt, mkT_d[j * 4 + cs, :, o0_:o0_ + nt])
                            feat_gemm(wb, wk, 512, spans, hkeys, epi)
                    elif name == 'mv':
                        def epi(pt, pk, i, j=j):
                            evac_store(pt, pk, 512, mv_d[gts[i], :, j * 512:(j + 1) * 512])
                        tok_gemm(wb, wk, 512, tiles, epi)
                    elif name == 'mo':
                        def epi(pt, pk, i, j=j):
                            evac_store(pt, pk, 512, mo_d[gts[i] - 34, :, j * 512:(j + 1) * 512], func=AF.Sigmoid)
                        tok_gemm(wb, wk, 512, tiles, epi)
                    elif name == 'aqT':
                        def epi(pt, pk, cs, si, nt, j=j):
                            o0_ = (gts[0] - 34) * 128 + si * 512
                            rope_store(pt, pk, nt, si * 512, aqT_d[j * 4 + cs, :, o0_:o0_ + nt])
                        feat_gemm(wb, wk, 512, spans, hkeys, epi)
                    elif name == 'mqT':
                        def epi(pt, pk, cs, si, nt, j=j):
                            o0_ = (gts[0] - 34) * 128 + si * 512
                            evac_store(pt, pk, nt, mqT_d[j * 4 + cs, :, o0_:o0_ + nt], func=AF.Copy, scale=128.0 ** -0.5)
                        feat_gemm(wb, wk, 512, spans, hkeys, epi)
                    elif name in ('sgaT', 'sgmT'):
                        dd = sgaT_d if name == 'sgaT' else sgmT_d
                        def epi(pt, pk, cs, si, nt, j=j, dd=dd):
                            o0_ = (gts[0] - 34) * 128 + si * 512
                            evac_store(pt, pk, nt, dd[j * 4 + cs, :, o0_:o0_ + nt], func=AF.Sigmoid)
                        feat_gemm(wb, wk, 512, spans, hkeys, epi)
                for _ in range(12):
                    lookahead()
            P.fence()


        if 'T' in PHASES:
            cv = Carve(big)
            KT = cv.bf(4 * 4480, 4480)
            V = cv.bf(35 * 512, 512)
            sinkE = cv.f32(D)
            QT = Rot([cv.bf(D) for _ in range(3)], 'QT')
            PTs = Rot([cv.bf(512) for _ in range(12)], 'PTs')
            dn = Rot([cv.f32(512) for _ in range(2)], 'dn')
            ost = Rot([cv.bf(512) for _ in range(2)], 'ost')
            for g in range(4):
                P.dma('sp', KT[:, g, :], akT_d[g], w=['KT'])
            P.dma('sp', V, av_d.rearrange("t p c -> p t c"), w=['V'])
            P.dma('sp', sinkE, sinkrep, w=['sk'])
            P.act(ACT(sinkE, sinkE, AF.Exp), r=['sk'], w=['sk'])
            PSs = Rot(ps[0:4], 'pss'); PSo = Rot(ps[4:6], 'pso'); PSd = Rot(ps[6:8], 'psd')
            NJ = int(os.environ.get("MK_TN", "32"))
            its = [(j, g) for j in range(NJ) for g in range(4)]
            qcur = {}

            def stageA(j, g):
                if g == 0:
                    q_, qk = QT.next()
                    P.dma('sp', q_.rearrange("p (h q) -> p h q", q=128),
                          aqT_d[:, :, j * 128:(j + 1) * 128].rearrange("h d q -> d h q"), w=[qk])
                    qcur[j] = (q_, qk)
                q_, qk = qcur[j]
                kbs = [(j, negB4), (j + 1, None)] + ([(j + 2, negA4)] if j < 31 else []) + [(33, None), (34, None)]
                pts = []
                for (kt, msk) in kbs:
                    s_, sk_ = PSs.next()
                    P.pe(MM(s_, KT[:, g, kt * 128:(kt + 1) * 128], q_[:, g * 512:(g + 1) * 512], True, msk is None), r=['KT', qk], w=[sk_])
                    if msk is not None:
                        P.pe(MM(s_, ident, msk, False, True), r=[], w=[sk_])
                    p_, pk_ = PTs.next()
                    P.act(ACT(p_, s_, AF.Exp, scale=128.0 ** -0.5), r=[sk_], w=[pk_])
                    pts.append((kt, p_, pk_))
                return pts

            def stageB(j, g, pts):
                po, pok = PSo.next(); pd, pdk = PSd.next()
                for idx, (kt, p_, pk_) in enumerate(pts):
                    first = idx == 0; last = idx == len(pts) - 1
                    P.pe(MM(po, V[:, kt, g * 128:(g + 1) * 128], p_, first, last), r=['V', pk_], w=[pok])
                    P.pe(MM(pd, ones_bf, p_, first, last), r=[pk_], w=[pdk])
                d_, dk_ = dn.next()
                P.dve(TT(d_, pd, sinkE[:, g * 512:(g + 1) * 512], ALU.add), r=[pdk, 'sk'], w=[dk_])
                P.dve(RCP(d_, d_), r=[dk_], w=[dk_])
                o_, ok_ = ost.next()
                P.dve(TT(o_, po, d_, ALU.mult), r=[pok, dk_], w=[ok_])
                P.dma('sp', attT_d[4 * g:4 * g + 4, :, j * 128:(j + 1) * 128].rearrange("h d q -> d h q"),
                      o_.rearrange("p (h q) -> p h q", q=128), r=[ok_], w=['dram'])
            prev = stageA(*its[0])
            for ii, (j, g) in enumerate(its):
                nxt = stageA(*its[ii + 1]) if ii + 1 < len(its) else None
                stageB(j, g, prev)
                prev = nxt
            P.fence()

        if 'M' in PHASES:
            cv = Carve(big)
            G = cv.f32(66 * 32, 32)
            lfc = [cv.f32(528), cv.f32(528)]
            bb = [cv.f32(528), cv.f32(528)]
            ee = [cv.f32(528, 8), cv.f32(528, 8)]
            uu = [cv.f32(528).rearrange("p (t h o) -> p t h o", h=8, o=1) for _ in range(2)]
            th = [cv.f32(528, 8), cv.f32(528, 8)]
            dtmp = cv.f32(528)
            Cst = [cv.f32(8 * 257, 257) for _ in range(2)]
            CbfA = cv.bf(8 * 257 + 8)[:, 0:8 * 257].rearrange("p (h c) -> p h c", c=257)
            CbfB = Rot([cv.bf(8 * 257 + 8)[:, 0:8 * 257] for _ in range(2)], 'CbfB')
            mkt = Rot([cv.bf(1024) for _ in range(4)], 'mkt')
            mvt = Rot([cv.bf(2048, 256) for _ in range(4)], 'mvt')
            vEr = [Rot([cv.bf(8 * 257 + 8)[:, 0:8 * 257].rearrange("p (h c) -> p h c", c=257) for _ in range(2)], 'vE%d' % d) for d in range(2)]
            tmpE = Rot([cv.f32(257) for _ in range(3)], 'tmpE')
            qTr = Rot([cv.bf(1024, 128) for _ in range(2)], 'qTr')
            kTr = Rot([cv.bf(1024, 128) for _ in range(2)], 'kTr')
            mot = Rot([cv.bf(2048) for _ in range(2)], 'mot')
            Sb = Rot([cv.bf(128) for _ in range(2)], 'Sb')
            SmAr = Rot([cv.bf(128) for _ in range(2)], 'SmA')
            SmBr = Rot([cv.bf(128) for _ in range(2)], 'SmB')
            hbuf = cv.f32(2048); gmo = cv.f32(2048); gm = cv.f32(2048)
            hA = Rot([cv.f32(256) for _ in range(2)], 'hA')
            junk = cv.bf(256)
            memr = Rot([cv.bf(2048) for _ in range(2)], 'mem')
            memTs = Rot([cv.bf(2048, 128) for _ in range(2)], 'memTs')
            d2r = Rot([cv.f32(2) for _ in range(6)], 'd2')
            ssm = Rot([cv.f32(8) for _ in range(2)], 'ssm')
            P.dma('sp', G, gs_d.rearrange("t p c -> p t c"), w=['G'])
            P.dma('sp', gm, gmrep, w=['gm'])
            for d in range(2):
                P.dve(CP(lfc[d].rearrange("p (t h) -> p t h", h=8), G[:, :, d * 16 + 8:d * 16 + 16]), r=['G'], w=[('lfc', d)])
            PSr = Rot(ps[0:6], 'psm')
            for d in range(2):
                tri = triA_f if d == 0 else triB_f
                for hf in range(2):
                    c0 = hf * 264
                    pb, pbk = PSr.next(); pe2, pek = PSr.next()
                    P.pe(MM(pb[:, :264], tri, lfc[d][:, c0:c0 + 264]), r=[('lfc', d)], w=[pbk])
                    P.pe(MM(pe2[:, :264], ones_f, lfc[d][:, c0:c0 + 264]), r=[('lfc', d)], w=[pek])
                    thv = th[d].rearrange("p t h -> p (t h)")[:, c0:c0 + 264]
                    eev = ee[d].rearrange("p t h -> p (t h)")[:, c0:c0 + 264]
                    uuv = uu[d].rearrange("p t h o -> p (t h o)")[:, c0:c0 + 264]
                    P.act(ACT(thv, pb[:, :264], AF.Exp, scale=-1.0), r=[pbk], w=[('th', d)])
                    P.act(ACT(eev, pe2[:, :264], AF.Exp), r=[pek], w=[('ee', d)])
                    P.dve(TT(dtmp[:, c0:c0 + 264].rearrange("p (t h) -> p t h", h=8), G[:, hf * 33:hf * 33 + 33, d * 16:d * 16 + 8],
                             pb[:, :264].rearrange("p (t h) -> p t h", h=8), ALU.subtract), r=['G', pbk], w=['dtmp'])
                    P.act(ACT(uuv, dtmp[:, c0:c0 + 264], AF.Exp), r=['dtmp'], w=[('uu', d)])
            for d in range(2):
                P.dve(MEMSET(Cst[d].rearrange("p h c -> p (h c)"), 0.0), w=[('C', d, h) for h in range(8)])
            P.dve(MEMSET(CbfA.rearrange("p h c -> p (h c)"), 0.0), w=[('CbfA', h) for h in range(8)])

            def load_kv(n):
                mk_, mkk = mkt.next(); mv_, mvk = mvt.next()
                P.dma('sp', mk_, mk_d[n], w=[mkk])
                P.dma('sp', mv_.rearrange("p h c -> p (h c)"), mv_d[n], w=[mvk])
                return mk_, mkk, mv_, mvk

            def make_vE(d, n, mv_, mvk):
                v_, vk = vEr[d].next()
                P.dve(TT(v_[:, :, 0:256], mv_, uu[d][:, n].to_broadcast([128, 8, 256]), ALU.mult), r=[mvk, ('uu', d)], w=[vk])
                P.act(ACT(v_[:, :, 256:257], uu[d][:, n], AF.Copy), r=[('uu', d)], w=[vk])
                return v_, vk

            def state_update(d, n, h, mk_, mkk, v_, vk, PSc, cbf=None, cbfk=None):
                cl, clk = PSc.next()
                P.pe(MM(cl[:, :257], mk_[:, h * 128:(h + 1) * 128], v_[:, h, :]), r=[mkk, vk], w=[clk])
                t_, tk = tmpE.next()
                e_ap = ee[d][:, n, h:h + 1]
                P.act(ACT(t_, cl[:, :257], AF.Copy, scale=e_ap), r=[clk, ('ee', d)], w=[tk])
                P.dve(STT(Cst[d][:, h, :], Cst[d][:, h, :], e_ap, t_, ALU.mult, ALU.add), r=[tk, ('ee', d)], w=[('C', d, h)])
                if cbf is not None:
                    P.pool(CP(cbf[:, h, :], Cst[d][:, h, :]), r=[('C', d, h)], w=[cbfk(h)])

            full = not os.environ.get("MK_PSHORT")
            preT = list(range(2, 34)) if full else list(range(26, 34))
            ownT = list(range(34, 66)) if full else list(range(34, 42))
            def stepA(n):
                mk_, mkk, mv_, mvk = load_kv(n)
                v_, vk = make_vE(0, n, mv_, mvk)
                last = (n == preT[-1])
                for h in range(8):
                    state_update(0, n, h, mk_, mkk, v_, vk, PSr, CbfA if last else None, (lambda h: ('CbfA', h)))

            def stepB(n):
                mk_, mkk, mv_, mvk = load_kv(n)
                v_, vk = make_vE(1, n, mv_, mvk)
                if n >= 34:
                    cb_, cbk = CbfB.next()
                    P.pool(CP(cb_, Cst[1].rearrange("p h c -> p (h c)")), r=[('C', 1, h) for h in range(8)], w=[cbk])
                    P.dma('sp', stB_d[n - 34], cb_, r=[cbk], w=['dram_st'])
                for h in range(8):
                    state_update(1, n, h, mk_, mkk, v_, vk, PSr)
            la_, lb_ = [0, 1] + preT, [1, 0] + ownT[::-1]
            for ii in range(max(len(la_), len(lb_))):
                if ii < len(la_):
                    stepA(la_[ii])
                if ii < len(lb_):
                    stepB(lb_[ii])
            P.fence()
            S4 = Rot([ps[0][:, i * 128:(i + 1) * 128] for i in range(4)], 'S4')
            PNA = Rot(ps[1:3], 'pna'); PNB = Rot(ps[3:5], 'pnb'); PCL = Rot(ps[5:6], 'pcl'); PTm = Rot(psb[6:8], 'ptm')
            tstate = {}

            def prologue(n):
                j = n - 34
                mk_, mkk, mv_, mvk = load_kv(n)
                q_, qk = qTr.next(); k_, kk = kTr.next(); mo_, mok = mot.next(); cb_, cbk = CbfB.next()
                P.dma('sp', q_, mqT_d[:, :, j * 128:(j + 1) * 128].rearrange("h d t -> d h t"), w=[qk])
                P.dma('sp', k_, mkT_d[:, :, j * 128:(j + 1) * 128].rearrange("h d t -> d h t"), w=[kk])
                P.dma('sp', mo_, mo_d[j], w=[mok])
                P.dma('sp', cb_, stB_d[j], r=['dram_st'], w=[cbk])
                vA, vAk = make_vE(0, n, mv_, mvk)
                vB, vBk = make_vE(1, n, mv_, mvk)
                ss_, ssk = ssm.next()
                tstate[n] = dict(mk=(mk_, mkk), q=(q_, qk), k=(k_, kk), mo=(mo_, mok), cb=(cb_, cbk), vA=(vA, vAk), vB=(vB, vBk),
                                 ss=(ss_, ssk), sm={})

            def partA(n, h):
                if n not in tstate:
                    prologue(n)
                t_ = tstate[n]
                q_, qk = t_['q']; k_, kk = t_['k']
                s_, sk_ = S4.next()
                P.pe(MM(s_, k_[:, h, :], q_[:, h, :]), r=[kk, qk], w=[sk_])
                sb_, sbk = Sb.next()
                P.act(ACT(sb_, s_, AF.Copy), r=[sk_], w=[sbk])
                sa_, sak = SmAr.next(); sB_, sBk = SmBr.next()
                P.pool(TT(sa_, sb_, triA4[:, 0:128], ALU.mult), r=[sbk], w=[sak])
                P.pool(TT(sB_, sb_, triB4[:, 0:128], ALU.mult), r=[sbk], w=[sBk])
                t_['sm'][h] = (sa_, sak, sB_, sBk)

            def partB(n, h):
                t_ = tstate[n]
                mk_, mkk = t_['mk']; q_, qk = t_['q']; cb_, cbk = t_['cb']; vA, vAk = t_['vA']; vB, vBk = t_['vB']
                ss_, ssk = t_['ss']
                sa_, sak, sB_, sBk = t_['sm'][h]
                cb3 = cb_.rearrange("p (h c) -> p h c", c=257)
                na, nak = PNA.next(); nb_, nbk = PNB.next()
                P.pe(MM(na[:, :257], sa_, vA[:, h, :], True, False), r=[sak, vAk], w=[nak])
                P.pe(MM(na[:, :257], q_[:, h, :], CbfA[:, h, :], False, True), r=[qk, ('CbfA', h)], w=[nak])
                P.pe(MM(nb_[:, :257], sB_, vB[:, h, :], True, False), r=[sBk, vBk], w=[nbk])
                P.pe(MM(nb_[:, :257], q_[:, h, :], cb3[:, h, :], False, True), r=[qk, cbk], w=[nbk])
                d2, d2k = d2r.next()
                P.dve(TS(d2[:, 0:1], na[:, 256:257], -1.0, na[:, 256:257], ALU.mult, ALU.max), r=[nak], w=[d2k])
                P.dve(TT(d2[:, 0:1], d2[:, 0:1], th[0][:, n, h:h + 1], ALU.max), r=[d2k, ('th', 0)], w=[d2k])
                P.dve(TS(d2[:, 1:2], nb_[:, 256:257], -1.0, nb_[:, 256:257], ALU.mult, ALU.max), r=[nbk], w=[d2k])
                P.dve(TT(d2[:, 1:2], d2[:, 1:2], th[1][:, n, h:h + 1], ALU.max), r=[d2k, ('th', 1)], w=[d2k])
                P.dve(RCP(d2, d2), r=[d2k], w=[d2k])
                ha_, hak = hA.next()
                P.act(ACT(ha_, na[:, 0:256], AF.Copy, scale=d2[:, 0:1]), r=[nak, d2k], w=[hak])
                P.dve(STT(hbuf[:, h * 256:(h + 1) * 256], nb_[:, 0:256], d2[:, 1:2], ha_, ALU.mult, ALU.add),
                      r=[nbk, d2k, hak], w=[('hbuf', h)])
                P.act(ACT(junk, hbuf[:, h * 256:(h + 1) * 256], AF.Square, accum_out=ss_[:, h:h + 1]), r=[('hbuf', h)], w=['junkm', ssk])
                state_update(0, n, h, mk_, mkk, vA, vAk, PCL, CbfA, (lambda h: ('CbfA', h)))

            def epilogue(n):
                j = n - 34
                t_ = tstate.pop(n)
                mo_, mok = t_['mo']; ss_, ssk = t_['ss']
                P.pool(TT(gmo, mo_, gm, ALU.mult), r=[mok, 'gm'], w=['gmo'])
                P.dve(TS(ss_, ss_, 1.0 / 256, EPS, ALU.mult, ALU.add), r=[ssk], w=[ssk])
                P.act(ACT(ss_, ss_, AF.Sqrt), r=[ssk], w=[ssk])
                P.dve(RCP(ss_, ss_), r=[ssk], w=[ssk])
                m_, mk2 = memr.next()
                for h in range(8):
                    P.dve(STT(m_[:, h * 256:(h + 1) * 256], hbuf[:, h * 256:(h + 1) * 256], ss_[:, h:h + 1],
                              gmo[:, h * 256:(h + 1) * 256], ALU.mult, ALU.mult), r=[('hbuf', h), ssk, 'gmo'], w=[mk2])
                mt_, mtk = memTs.next()
                for half in range(2):
                    pt, pk = PTm.next()
                    for jj in range(8):
                        kc = half * 8 + jj
                        P.pe(TRP(pt[:, jj * 128:(jj + 1) * 128], m_[:, kc * 128:(kc + 1) * 128], ident), r=[mk2], w=[pk])
                    P.act(ACT(mt_[:, half * 8:half * 8 + 8, :], pt.rearrange("p (a b) -> p a b", b=128), AF.Copy), r=[pk], w=[mtk])
                P.dma('sp', memT_d[:, :, j * 128:(j + 1) * 128].rearrange("c p t -> p c t"), mt_, r=[mtk], w=['dram'])
            seq = [(n, h) for n in ownT for h in range(8)]
            partA(*seq[0])
            for ii, (n, h) in enumerate(seq):
                if ii + 1 < len(seq):
                    partA(*seq[ii + 1])
                partB(n, h)
                if h == 7:
                    epilogue(n)
            P.fence()

        if 'G' in PHASES:
            cv = Carve(big)
            attT = cv.bf(16 * 512, 512); memT = cv.bf(16 * 512, 512)
            x2b = big[:, 0:4 * D].rearrange("p (i c) -> p i c", c=D)
            yT = cv.bf(16 * 512, 512)
            sgr = Rot([cv.bf(512) for _ in range(4)], 'sg')
            tmp = cv.f32(D)
            hb = Rot([cv.bf(D) for _ in range(2)], 'hb')
            junk = cv.bf(D)
            G1 = cv.f32(D); A2 = cv.f32(D); B2 = cv.f32(D)
            WB = [cv.bf(16 * 512, 512) for _ in range(4)]
            h2Ts = Rot([cv.bf(2048, 128) for _ in range(2)], 'h2Ts')
            t1 = Rot([cv.f32(512) for _ in range(2)], 't1')
            t2 = Rot([cv.f32(512) for _ in range(2)], 't2')
            xp = Rot([cv.f32(512) for _ in range(3)], 'xp')
            ssq = Rot([cv.f32(1) for _ in range(4)], 'ssq')
            P.dma('sp', G1, modsc[2], w=['G1']); P.dma('sp', A2, modsc[3], w=['A2']); P.dma('sp', B2, modsc[4], w=['B2'])
            vap = wview(q_ap); vmp = wview(q_mp); vo = wview(q_o)
            PS = Rot(ps[0:6], 'ps'); PT = Rot(psb[6:8], 'pt')
            nst = int(os.environ.get("MK_GN", "8"))
            blocks = []
            for st in range(nst):
                for fg in range(4):
                    blocks.append((vap, 0, 16, fg * 512, 512)); blocks.append((vmp, 0, 16, fg * 512, 512))
                for nb in range(4):
                    blocks.append((vo, 0, 16, nb * 512, 512))
            conv_issue(1000)
            ws = WStream(WB, blocks, rk=wqkeys['ap'] + wqkeys['mp'] + wqkeys['o'])
            for st in range(nst):
                o0 = st * 512
                P.dma('sp', attT, attT_d[:, :, o0:o0 + 512].rearrange("c p t -> p c t"), w=['R1'])
                P.dma('sp', memT, memT_d[:, :, o0:o0 + 512].rearrange("c p t -> p c t"), w=['R1'])
                for fg in range(4):
                    wa, wak = ws.get(st * 12 + 2 * fg, 2); wm, wmk = ws.get(st * 12 + 2 * fg + 1, 2)
                    for cs in range(4):
                        fc = fg * 4 + cs
                        pa, pak = PS.next(); pm2, pmk = PS.next()
                        for kc in range(16):
                            P.pe(MM(pa, wa[:, kc, cs * 128:(cs + 1) * 128], attT[:, kc, :], kc == 0, kc == 15), r=[wak, 'R1'], w=[pak])
                        for kc in range(16):
                            P.pe(MM(pm2, wm[:, kc, cs * 128:(cs + 1) * 128], memT[:, kc, :], kc == 0, kc == 15), r=[wmk, 'R1'], w=[pmk])
                        sa_, sak = sgr.next(); sm_, smk = sgr.next()
                        P.dma('sp', sa_, sgaT_d[fc, :, o0:o0 + 512], w=[sak])
                        P.dma('sp', sm_, sgmT_d[fc, :, o0:o0 + 512], w=[smk])
                        a_, ak = t1.next(); b_, bk = t2.next()
                        P.dve(TT(a_, pa, sa_, ALU.mult), r=[pak, sak], w=[ak])
                        P.dve(TT(b_, pm2, sm_, ALU.mult), r=[pmk, smk], w=[bk])
                        P.dve(TT(yT[:, fc, :], a_, b_, ALU.add), r=[ak, bk], w=[('yT', fc)])
                ykeys = [('yT', fc) for fc in range(16)]
                for nb in range(4):
                    wo, wok = ws.get(st * 12 + 8 + nb)
                    for i in range(4):
                        pt, pk = PS.next()
                        for kc in range(16):
                            P.pe(MM(pt, yT[:, kc, i * 128:(i + 1) * 128], wo[:, kc, :], kc == 0, kc == 15), r=[wok] + ykeys, w=[pk])
                        x_, xk = xp.next()
                        tok0 = TOWN + o0 + i * 128
                        P.dma('sp', x_, xv[tok0:tok0 + 128, nb * 512:(nb + 1) * 512], w=[xk])
                        a_, ak = t1.next()
                        P.dve(TT(a_, pt, G1[:, nb * 512:(nb + 1) * 512], ALU.mult), r=[pk, 'G1'], w=[ak])
                        P.dve(TT(x2b[:, i, nb * 512:(nb + 1) * 512], a_, x_, ALU.add), r=[ak, xk], w=['R1'])
                for i in range(4):
                    gt = st * 4 + i
                    P.dma('sp', x2_d[gt], x2b[:, i, :], r=['R1'], w=['dram'])
                    s1, s1k = ssq.next()
                    P.act(ACT(junk, x2b[:, i, :], AF.Square, accum_out=s1), r=['R1'], w=['junk', s1k])
                    P.dve(TS(s1, s1, 1.0 / D, EPS, ALU.mult, ALU.add), r=[s1k], w=[s1k])
                    P.act(ACT(s1, s1, AF.Sqrt), r=[s1k], w=[s1k])
                    P.dve(RCP(s1, s1), r=[s1k], w=[s1k])
                    P.dve(STT(tmp, x2b[:, i, :], s1, A2, ALU.mult, ALU.mult), r=['R1', s1k, 'A2'], w=['tmp'])
                    h_, hk = hb.next()
                    P.dve(TT(h_, tmp, B2, ALU.add), r=['tmp', 'B2'], w=[hk])
                    ht_, htk = h2Ts.next()
                    for half in range(2):
                        pt, pk = PT.next()
                        for jj in range(8):
                            kc = half * 8 + jj
                            P.pe(TRP(pt[:, jj * 128:(jj + 1) * 128], h_[:, kc * 128:(kc + 1) * 128], ident), r=[hk], w=[pk])
                        P.act(ACT(ht_[:, half * 8:half * 8 + 8, :], pt.rearrange("p (a b) -> p a b", b=128), AF.Copy), r=[pk], w=[htk])
                    P.dma('sp', h2T_d[:, :, gt * 128:(gt + 1) * 128].rearrange("c p t -> p c t"), ht_, r=[htk], w=['dram'])
            P.fence()

        if 'F' in PHASES:
            cv = Carve(big)
            h2T = cv.bf(16 * 512, 512)
            actT = cv.bf(44 * 512, 512)
            WB = [cv.bf(16 * 512, 512) for _ in range(4)]
            x3 = cv.f32(4 * D, D)
            G2 = cv.f32(D); gF = cv.f32(D)
            t1 = Rot([cv.f32(512) for _ in range(2)], 't1')
            junk = cv.bf(D)
            ssq = Rot([cv.f32(1) for _ in range(4)], 'ssq')
            P.dma('sp', G2, modsc[5], w=['G2']); P.dma('sp', gF, gfrep, w=['gF'])
            vfi = wview(q_fi); vfo = wview(q_fo)
            PS = Rot(ps[0:4], 'ps')
            nst = int(os.environ.get("MK_GN", "8"))
            blocks = []
            for st in range(nst):
                for fg in range(11):
                    blocks.append((vfi, 0, 16, fg * 512, 512)); blocks.append((vfi, 0, 16, DFF + fg * 512, 512))
                for nb in range(4):
                    for kg in range(4):
                        blocks.append((vfo, kg * 11, 11, nb * 512, 512))
            conv_issue(1000)
            ws = WStream(WB, blocks, rk=wqkeys['fi'] + wqkeys['fo'])
            for st in range(nst):
                o0 = st * 512
                P.dma('sp', h2T, h2T_d[:, :, o0:o0 + 512].rearrange("c p t -> p c t"), w=['h2T'])
                for i in range(4):
                    P.dma('sp', x3[:, i, :], x2_d[st * 4 + i], w=[('x3', i)])
                for fg in range(11):
                    wg_, wgk = ws.get(st * 38 + 2 * fg, 2); wu_, wuk = ws.get(st * 38 + 2 * fg + 1, 2)
                    for cs in range(4):
                        fc = fg * 4 + cs
                        pg, pgk = PS.next(); pu, puk = PS.next()
                        for kc in range(16):
                            P.pe(MM(pg, wg_[:, kc, cs * 128:(cs + 1) * 128], h2T[:, kc, :], kc == 0, kc == 15), r=[wgk, 'h2T'], w=[pgk])
                        for kc in range(16):
                            P.pe(MM(pu, wu_[:, kc, cs * 128:(cs + 1) * 128], h2T[:, kc, :], kc == 0, kc == 15), r=[wuk, 'h2T'], w=[puk])
                        a_, ak = t1.next()
                        P.act(ACT(a_, pg, AF.Silu), r=[pgk], w=[ak])
                        P.dve(TT(actT[:, fc, :], a_, pu, ALU.mult), r=[ak, puk], w=[('actT', fc)])
                akeys = [('actT', fc) for fc in range(44)]
                for nb in range(4):
                    for kg in range(4):
                        wo, wok = ws.get(st * 38 + 22 + nb * 4 + kg)
                        for i in range(4):
                            for k in range(11):
                                kc = kg * 11 + k
                                P.pe(MM(ps[4 + i], actT[:, kc, i * 128:(i + 1) * 128], wo[:, k, :], kg == 0 and k == 0, kg == 3 and k == 10),
                                     r=[wok] + akeys, w=[('acc', i)])
                    for i in range(4):
                        a_, ak = t1.next()
                        P.dve(TT(a_, ps[4 + i], G2[:, nb * 512:(nb + 1) * 512], ALU.mult), r=[('acc', i), 'G2'], w=[ak])
                        P.dve(TT(x3[:, i, nb * 512:(nb + 1) * 512], a_, x3[:, i, nb * 512:(nb + 1) * 512], ALU.add), r=[ak, ('x3', i)], w=[('x3', i)])
                for i in range(4):
                    gt = st * 4 + i
                    s1, s1k = ssq.next()
                    P.act(ACT(junk, x3[:, i, :], AF.Square, accum_out=s1), r=[('x3', i)], w=['junk', s1k])
                    P.dve(TS(s1, s1, 1.0 / D, EPS, ALU.mult, ALU.add), r=[s1k], w=[s1k])
                    P.act(ACT(s1, s1, AF.Sqrt), r=[s1k], w=[s1k])
                    P.dve(RCP(s1, s1), r=[s1k], w=[s1k])
                    P.dve(STT(x3[:, i, :], x3[:, i, :], s1, gF, ALU.mult, ALU.mult), r=[('x3', i), s1k, 'gF'], w=[('x3', i)])
                    P.dma('sp', out[gt * 128:(gt + 1) * 128, :], x3[:, i, :], r=[('x3', i)], w=['out'])
            P.fence()

        P.resolve(sems, dsems)
        with nc.Block() as block:
            @block.tensor
            def _(e):
                P.emit('pe', e)

            @block.scalar
            def _(e):
                P.emit('act', e)

            @block.vector
            def _(e):
                P.emit('dve', e)

            @block.gpsimd
            def _(e):
                P.emit('pool', e)

            @block.sync
            def _(e):
                P.emit('sp', e, final_wait=True)
    return nc


def _rep(v, n=128):
    return np.ascontiguousarray(np.broadcast_to(np.asarray(v, np.float32).reshape(1, -1), (n, v.size)))


def make_in_maps(x, c, ctx, c_ctx, w_ada, b_ada, norm1_g, w_in, b_gates, attn_sink, mlstm_norm_g,
                 w_attn_proj, w_mlstm_proj, w_out, norm2_g, w_ffn_in, w_ffn_out, final_norm_g):
    f = np.float32
    x = np.asarray(x, f); ctx = np.asarray(ctx, f); c = np.asarray(c, f); c_ctx = np.asarray(c_ctx, f)
    W = dict(w_ada=np.ascontiguousarray(np.asarray(w_ada, f)[0]), w_in=np.ascontiguousarray(np.asarray(w_in, f)[0]),
             w_ap=np.ascontiguousarray(np.asarray(w_attn_proj, f)[0]), w_mp=np.ascontiguousarray(np.asarray(w_mlstm_proj, f)[0]),
             w_o=np.ascontiguousarray(np.asarray(w_out, f)[0]), w_fi=np.ascontiguousarray(np.asarray(w_ffn_in, f)[0]),
             w_fo=np.ascontiguousarray(np.asarray(w_ffn_out, f)[0]))
    shared = dict(W)
    shared['bada_rep'] = _rep(np.asarray(b_ada, f)[0])
    shared['g1rep'] = _rep(np.asarray(norm1_g, f)[0]); shared['g2rep'] = _rep(np.asarray(norm2_g, f)[0])
    shared['gfrep'] = _rep(np.asarray(final_norm_g, f)); shared['gmrep'] = _rep(np.asarray(mlstm_norm_g, f)[0])
    shared['sinkrep'] = _rep(np.repeat(np.asarray(attn_sink, f)[0], 128))
    shared['cident'] = np.eye(128, dtype=f)
    pm = np.zeros((128, 128), f)
    for j in range(128):
        partner = j + 32 if (j % 64) < 32 else j - 32
        pm[partner, j] = 1.0
    shared['cpm'] = pm
    s_ = np.arange(128)
    triA = (s_[:, None] <= s_[None, :]).astype(f); triB = (s_[:, None] >= s_[None, :]).astype(f)
    shared['ctriA'] = np.ascontiguousarray(np.tile(triA, (1, 4))); shared['ctriB'] = np.ascontiguousarray(np.tile(triB, (1, 4)))
    shared['cones'] = np.ones((128, 128), f)
    shared['cnegA'] = np.ascontiguousarray(np.tile(np.where(triA > 0, 0.0, -30000.0).astype(f), (1, 4)))
    shared['cnegB'] = np.ascontiguousarray(np.tile(np.where(triB > 0, 0.0, -30000.0).astype(f), (1, 4)))

    def crepf(v):
        return np.ascontiguousarray(np.broadcast_to(v.reshape(16, 128).T[:, :, None], (128, 16, 128)).reshape(128, D))
    shared['cctxrep'] = crepf(c_ctx)
    wg_full = W['w_in'][:, C_MG:C_MG + 32]
    bgf = np.asarray(b_gates, f)[0]
    inv_freq = (np.float32(10000.0) ** (-np.arange(32, dtype=f) / np.float32(32))).astype(f)
    jj = np.arange(128)
    in_maps = []
    for core in range(8):
        b, half = core // 2, core % 2
        flip = (half == 0)
        m = dict(shared)
        m['xv'] = np.ascontiguousarray(x[b][::-1]) if flip else np.ascontiguousarray(x[b])
        m['ctxv'] = np.ascontiguousarray(ctx[b][::-1]) if flip else np.ascontiguousarray(ctx[b])
        m['crep'] = crepf(c[b])
        dA = 1 if flip else 0
        order = list(range(dA * 16, dA * 16 + 16)) + list(range((1 - dA) * 16, (1 - dA) * 16 + 16))
        m['w_g'] = np.ascontiguousarray(wg_full[:, order]); m['bg_rep'] = _rep(bgf[order])
        tpos = np.arange(TOWN - 128, SEQ)
        torig = (SEQ - 1 - tpos) if flip else tpos
        rows = (torig // 64).astype(f); cols = (torig % 64).astype(f)
        pos = np.where((jj < 64)[:, None], rows[None, :], cols[None, :]).astype(f)
        ang = (pos * inv_freq[jj % 32][:, None]).astype(f)
        sgn = np.where((jj % 64) < 32, -1.0, 1.0).astype(f)[:, None]
        m['cosT'] = np.ascontiguousarray(np.cos(ang).astype(f)); m['sinT'] = np.ascontiguousarray((np.sin(ang) * sgn).astype(f))
        in_maps.append(m)
    return in_maps


def kernel(**inputs):
    in_maps = make_in_maps(**inputs)
    nc = build()
    res = run_bass_kernel_spmd(nc, in_maps, core_ids=list(range(8)))
    outp = np.empty((4, SEQ, D), np.float32)
    for core in range(8):
        b, half = core // 2, core % 2
        o = np.asarray(res.results[core]["out"], np.float32)
        if half == 0:
            outp[b, 0:TOWN] = o[::-1]
        else:
            outp[b, TOWN:SEQ] = o
    return outp
```

```python
import os
import numpy as np
import concourse.bass as bass
import concourse.mybir as mybir
from concourse.bass_utils import run_bass_kernel_spmd

F32 = mybir.dt.float32
BF16 = mybir.dt.bfloat16
AF = mybir.ActivationFunctionType
ALU = mybir.AluOpType

D = 2048
KC = 16
SEQ = 8192
TOWN = 4096
NTO = 32
CTXL = 256
DFF = 5632
EPS = 1e-6
C_AK, C_AV, C_MK, C_MV, C_MG, C_AQ, C_MQ, C_MO, C_GA, C_GM = 0, 512, 1024, 2048, 4096, 4128, 6176, 7200, 9248, 11296
NIN = 13344
NS = 20
BIGW = 47 * 1024
ENG = ['pe', 'act', 'dve', 'pool', 'sp']
PHASES = os.environ.get("MK_PHASES", "APTMGF")


class Op:
    __slots__ = ('eng', 'fn', 'dma', 'needed', 'val', 'deps', 'dsem', 'dval', 'grp')


class Prog:
    def __init__(self):
        self.ops = {e: [] for e in ENG}
        self.lastw = {}
        self.rd = {}
        self.fence_deps = None
        self.fenced = set()
        self.dmas = []
        self.lastc = {}

    def op(self, eng, fn, r=(), w=(), dma=False, grp=None):
        o = Op()
        o.eng = eng; o.fn = fn; o.dma = dma; o.needed = False; o.val = None; o.grp = grp or eng
        deps = []
        for k in r:
            lw = self.lastw.get(k)
            if lw is not None:
                deps.append(lw)
        for k in w:
            lw = self.lastw.get(k)
            if lw is not None:
                deps.append(lw)
            rr = self.rd.get(k)
            if rr:
                deps.extend(rr[0].values()); deps.extend(rr[1])
        if self.fence_deps is not None and eng not in self.fenced:
            deps.extend(self.fence_deps); self.fenced.add(eng)
        for k in r:
            rr = self.rd.setdefault(k, ({}, []))
            if dma:
                rr[1].append(o)
            else:
                rr[0][eng] = o
        for k in w:
            self.lastw[k] = o
            self.rd[k] = ({}, [])
        o.deps = [d for d in deps if d is not o]
        for d in o.deps:
            d.needed = True
        self.ops[eng].append(o)
        if dma:
            if grp != 'cv':
                self.dmas.append(o)
        else:
            self.lastc[eng] = o
        return o

    def fence(self):
        f = list(self.lastc.values()) + self.dmas
        self.fence_deps = f
        self.fenced = set()
        self.dmas = []
        self.lastw = {k: v for k, v in self.lastw.items() if isinstance(k, tuple) and k[0] == 'wq'}
        self.rd = {k: v for k, v in self.rd.items() if isinstance(k, tuple) and k[0] == 'wq'}

    def pe(self, fn, r=(), w=()): return self.op('pe', fn, r, w)
    def act(self, fn, r=(), w=()): return self.op('act', fn, r, w)
    def dve(self, fn, r=(), w=()): return self.op('dve', fn, r, w)
    def pool(self, fn, r=(), w=()): return self.op('pool', fn, r, w)

    def dma(self, q, out, in_, r=(), w=(), grp=None):
        return self.op(q, lambda e: e.dma_start(out=out, in_=in_), r, w, dma=True, grp=grp)

    def resolve(self, sems, dsems):
        self.sems = sems
        self.final = {}
        for e in ENG:
            c = 0; ndg = {}
            for o in self.ops[e]:
                if o.dma:
                    nd = ndg.get(o.grp, 0); ndg[o.grp] = nd + 1
                    pool_ = dsems[o.grp]
                    o.dsem = pool_[nd % len(pool_)]; o.dval = 16 * (nd // len(pool_) + 1)
                    self.final[id(o.dsem)] = (o.dsem, o.dval)
                elif o.needed:
                    c += 1; o.val = c

    def emit(self, e, eng, final_wait=False):
        waited = {}

        def wait(sem, val):
            if waited.get(id(sem), 0) >= val:
                return
            eng.wait_ge(sem, val); waited[id(sem)] = val
        for o in self.ops[e]:
            for d in o.deps:
                if d.dma:
                    wait(d.dsem, d.dval)
                else:
                    if d.eng == e and e == 'pe':
                        continue
                    wait(self.sems[d.eng], d.val)
            if o.dma and o.dval > 16:
                wait(o.dsem, o.dval - 16)
            ins = o.fn(eng)
            if o.dma:
                ins.then_inc(o.dsem, 16)
            elif o.needed:
                ins.then_inc(self.sems[e], 1)
        if final_wait:
            for sem, val in self.final.values():
                wait(sem, val)


def MM(out, lhsT, rhs, start=True, stop=True):
    return lambda e: e.matmul(out, lhsT, rhs, start=start, stop=stop)


def TRP(out, in_, ident):
    return lambda e: e.transpose(out, in_, ident)


def ACT(out, in_, func, **kw):
    return lambda e: e.activation(out=out, in_=in_, func=func, **kw)


def TT(out, a, b, op):
    return lambda e: e.tensor_tensor(out=out, in0=a, in1=b, op=op)


def STT(out, a, s, b, op0, op1):
    return lambda e: e.scalar_tensor_tensor(out=out, in0=a, scalar=s, in1=b, op0=op0, op1=op1)


def TS(out, a, s1, s2, op0, op1=None):
    if op1 is None:
        return lambda e: e.tensor_scalar(out=out, in0=a, scalar1=s1, scalar2=None, op0=op0)
    return lambda e: e.tensor_scalar(out=out, in0=a, scalar1=s1, scalar2=s2, op0=op0, op1=op1)


def CP(out, in_):
    return lambda e: e.tensor_copy(out=out, in_=in_)


def RCP(out, in_):
    return lambda e: e.reciprocal(out=out, in_=in_)


def MEMSET(ap, v):
    return lambda e: e.memset(ap, v)


class Carve:
    def __init__(self, big):
        self.big = big; self.off = 0

    def f32(self, n, inner=None):
        ap = self.big[:, self.off:self.off + n]; self.off += n
        assert self.off <= BIGW, self.off
        if inner:
            ap = ap.rearrange("p (a b) -> p a b", b=inner)
        return ap

    def bf(self, n, inner=None):
        w = (n + 1) // 2
        ap = self.big[:, self.off:self.off + w].bitcast(BF16); self.off += w
        assert self.off <= BIGW, self.off
        if inner:
            ap = ap.rearrange("p (a b) -> p a b", b=inner)
        return ap


class Rot:
    def __init__(self, items, name):
        self.items = items; self.name = name; self.i = 0

    def next(self):
        k = self.i % len(self.items); self.i += 1
        return self.items[k], (self.name, k)


def build(dbg=()):
    nc = bass.Bass("TRN2", target_bir_lowering=False)

    def din(name, shape, dt=F32):
        return nc.dram_tensor(name, list(shape), dt, kind="ExternalInput").ap()

    def dscr(name, shape, dt):
        kind = "ExternalOutput" if name in dbg else "Internal"
        return nc.dram_tensor(name, list(shape), dt, kind=kind).ap()

    xv = din("xv", [SEQ, D]); ctxv = din("ctxv", [CTXL, D])
    crep = din("crep", [128, D]); cctxrep = din("cctxrep", [128, D])
    w_ada = din("w_ada", [D, 6 * D]); bada_rep = din("bada_rep", [128, 6 * D])
    g1rep = din("g1rep", [128, D]); g2rep = din("g2rep", [128, D]); gfrep = din("gfrep", [128, D])
    gmrep = din("gmrep", [128, D])
    w_in = din("w_in", [D, NIN]); w_g = din("w_g", [D, 32]); bg_rep = din("bg_rep", [128, 32])
    sinkrep = din("sinkrep", [128, D])
    w_ap = din("w_ap", [D, D]); w_mp = din("w_mp", [D, D]); w_o = din("w_o", [D, D])
    w_fi = din("w_fi", [D, 2 * DFF]); w_fo = din("w_fo", [DFF, D])
    cident = din("cident", [128, 128]); cpm = din("cpm", [128, 128])
    ctriA = din("ctriA", [128, 512]); ctriB = din("ctriB", [128, 512]); cones = din("cones", [128, 128])
    cnegA = din("cnegA", [128, 512]); cnegB = din("cnegB", [128, 512])
    cosT = din("cosT", [128, TOWN + 128]); sinT = din("sinT", [128, TOWN + 128])
    out = nc.dram_tensor("out", [TOWN, D], F32, kind="ExternalOutput").ap()

    q_in = dscr("q_in", [D, NIN], BF16); q_g = dscr("q_g", [D, 32], BF16)
    q_ap = dscr("q_ap", [D, D], BF16); q_mp = dscr("q_mp", [D, D], BF16); q_o = dscr("q_o", [D, D], BF16)
    q_fi = dscr("q_fi", [D, 2 * DFF], BF16); q_fo = dscr("q_fo", [DFF, D], BF16)
    modsc = dscr("modsc", [8, 128, D], F32)
    aqT_d = dscr("aqT_d", [16, 128, TOWN], BF16)
    akT_d = dscr("akT_d", [4, 128, TOWN + 128 + CTXL], BF16)
    av_d = dscr("av_d", [35, 128, 512], BF16)
    mqT_d = dscr("mqT_d", [8, 128, TOWN], BF16); mkT_d = dscr("mkT_d", [8, 128, TOWN], BF16)
    mk_d = dscr("mk_d", [66, 128, 1024], BF16); mv_d = dscr("mv_d", [66, 128, 2048], BF16)
    gs_d = dscr("gs_d", [66, 128, 32], F32)
    mo_d = dscr("mo_d", [32, 128, D], BF16)
    sgaT_d = dscr("sgaT_d", [16, 128, TOWN], BF16); sgmT_d = dscr("sgmT_d", [16, 128, TOWN], BF16)
    attT_d = dscr("attT_d", [16, 128, TOWN], BF16); memT_d = dscr("memT_d", [16, 128, TOWN], BF16)
    stB_d = dscr("stB_d", [32, 128, 8 * 257], BF16)
    x2_d = dscr("x2_d", [32, 128, D], F32)
    h2T_d = dscr("h2T_d", [16, 128, TOWN], BF16)

    P = Prog()
    from contextlib import ExitStack
    with ExitStack() as es:
        bigt = es.enter_context(nc.sbuf_tensor("big", [128, BIGW], F32))
        cst = es.enter_context(nc.sbuf_tensor("cst", [128, 2048], F32))
        pst = [es.enter_context(nc.psum_tensor("ps%d" % i, [128, 512], F32)) for i in range(8)]
        sems = {e: es.enter_context(nc.semaphore("s_" + e)) for e in ENG}
        dsems = {e: [es.enter_context(nc.semaphore("d_%s%d" % (e, i))) for i in range(NS)] for e in ('sp', 'pool')}
        dsems['cv'] = [es.enter_context(nc.semaphore("d_cv%d" % i)) for i in range(8)]
        big = bigt[:, :]
        ps = [p[:, :] for p in pst]
        psb = [p[:, :].bitcast(BF16) for p in pst]
        cc = cst[:, :]
        ident = cc[:, 0:64].bitcast(BF16)
        pm_bf = cc[:, 64:128].bitcast(BF16)
        ones_bf = cc[:, 128:192].bitcast(BF16)
        triA4 = cc[:, 192:448].bitcast(BF16)
        triB4 = cc[:, 448:704].bitcast(BF16)
        triA_f = cc[:, 704:832]; triB_f = cc[:, 832:960]; ones_f = cc[:, 960:1088]
        negA4 = cc[:, 1088:1344].bitcast(BF16); negB4 = cc[:, 1344:1600].bitcast(BF16)
        for dst, src in ((ident, cident), (pm_bf, cpm), (ones_bf, cones), (triA4, ctriA), (triB4, ctriB), (negA4, cnegA), (negB4, cnegB)):
            P.dma('pool', dst, src, w=['const'])
        for dst, src in ((triA_f, ctriA[:, 0:128]), (triB_f, ctriB[:, 0:128]), (ones_f, cones)):
            P.dma('sp', dst, src, w=['const'])
        P.fence()
        conv_q = []
        wqkeys = {}
        for (nm, qd, wd, rows) in (('g', q_g, w_g, D), ('in', q_in, w_in, D), ('ap', q_ap, w_ap, D), ('mp', q_mp, w_mp, D),
                                   ('o', q_o, w_o, D), ('fi', q_fi, w_fi, D), ('fo', q_fo, w_fo, DFF)):
            ks = []
            for r0 in range(0, rows, 256):
                ks.append(('wq', nm, r0))
                conv_q.append((qd[r0:r0 + 256, :], wd[r0:r0 + 256, :], ('wq', nm, r0)))
            wqkeys[nm] = ks

        def conv_issue(n):
            for _ in range(n):
                if conv_q:
                    o_, i_, k_ = conv_q.pop(0)
                    P.dma('pool', o_, i_, w=[k_], grp='cv')
        conv_issue(16)

        def wview(q):
            return q.rearrange("(kc p) n -> p kc n", p=128)

        class WStream:
            def __init__(self, WB, blocks, q='pool', rk=(), conv_per=0):
                self.WB = WB; self.blocks = blocks; self.issued = 0; self.q = q; self.rk = list(rk); self.conv_per = conv_per

            def _issue(self):
                i = self.issued
                v, k0, nk, c0, ncol = self.blocks[i]
                sl = i % len(self.WB)
                P.dma(self.q, self.WB[sl][:, 0:nk, 0:ncol], v[:, k0:k0 + nk, c0:c0 + ncol], r=self.rk, w=[('WB', sl)])
                self.issued += 1
                conv_issue(self.conv_per)

            def get(self, i, hold=1):
                while self.issued < min(len(self.blocks), i - hold + 1 + len(self.WB)):
                    self._issue()
                sl = i % len(self.WB)
                return self.WB[sl], ('WB', sl)

        if 'A' in PHASES:
            cv = Carve(big)
            cf = cv.f32(D)
            lhs = [cv.bf(D), cv.bf(D)]
            WB = [cv.bf(16 * 512, 512) for _ in range(3)]
            brep = Rot([cv.f32(512) for _ in range(2)], 'brep')
            grep = Rot([cv.f32(512) for _ in range(2)], 'grep')
            stg = Rot([cv.f32(512) for _ in range(3)], 'stgA')
            PS = Rot(ps[0:4], 'ps')
            for which, src in enumerate((crep, cctxrep)):
                P.dma('sp', cf, src, w=['cf'])
                P.act(ACT(lhs[which], cf, AF.Silu), r=['cf'], w=[('lhs', which)])
            vada = wview(w_ada)
            ws = WStream(WB, [(vada, 0, 16, n * 512, 512) for n in range(24)])
            slotmap = {(0, 0): 1, (0, 1): 0, (0, 2): 2, (0, 3): 4, (0, 4): 3, (0, 5): 5, (1, 0): 7, (1, 1): 6}
            for n in range(24):
                wb, wk = ws.get(n)
                v, cb = n // 4, n % 4
                for which in ((0, 1) if n < 8 else (0,)):
                    pt, pk = PS.next()
                    for kc in range(16):
                        P.pe(MM(pt, lhs[which][:, kc * 128:(kc + 1) * 128], wb[:, kc, :], kc == 0, kc == 15),
                             r=[wk, ('lhs', which)], w=[pk])
                    br, bk = brep.next()
                    P.dma('sp', br, bada_rep[:, n * 512:(n + 1) * 512], w=[bk])
                    st, sk = stg.next()
                    P.dve(TT(st, pt, br, ALU.add), r=[pk, bk], w=[sk])
                    if v in (1, 4):
                        gr, gk = grep.next()
                        P.dma('sp', gr, (g1rep if v == 1 else g2rep)[:, cb * 512:(cb + 1) * 512], w=[gk])
                        P.dve(STT(st, st, 1.0, gr, ALU.add, ALU.mult), r=[sk, gk], w=[sk])
                    P.dma('sp', modsc[slotmap[(which, v)], :, cb * 512:(cb + 1) * 512], st, r=[sk], w=['modsc'])
            P.fence()

        if 'P' in PHASES:
            cv = Carve(big)
            TS_ = 1024
            Am = cv.f32(D); Bm = cv.f32(D)
            xt = Rot([cv.f32(D) for _ in range(2)], 'xt')
            tmp = cv.f32(D)
            hb = Rot([cv.bf(D) for _ in range(2)], 'hb')
            junk = cv.bf(D)
            ssq = cv.f32(80); rstd = cv.f32(80)
            hTs = [cv.bf(16 * TS_, TS_) for _ in range(2)]
            WB = [cv.bf(16 * 512, 512) for _ in range(3)]
            cosS = cv.f32(TS_); sinS = cv.f32(TS_)
            stg = Rot([cv.bf(512) for _ in range(3)], 'stg')
            qb = Rot([cv.bf(512) for _ in range(2)], 'qb')
            t1 = Rot([cv.f32(512) for _ in range(2)], 't1')
            t2 = Rot([cv.f32(512) for _ in range(2)], 't2')
            wg_bf = cv.bf(16 * 32, 32)
            bg = cv.f32(32)
            gz = Rot([cv.f32(32) for _ in range(2)], 'gz')
            ge = Rot([cv.f32(16) for _ in range(2)], 'ge')
            PS = Rot(ps[0:6], 'ps')
            PT = Rot(psb[6:8], 'pt')
            P.dma('sp', wg_bf, wview(q_g), r=wqkeys['g'], w=['wg'])
            P.dma('sp', bg, bg_rep, w=['bg'])
            vin = wview(q_in)
            tcount = [0]
            evac_rr = [0]

            def n1_part1(xsrc):
                c = tcount[0] % 80; tcount[0] += 1
                x_, xk = xt.next()
                P.dma('sp', x_, xsrc, w=[xk])
                P.act(ACT(junk, x_, AF.Square, accum_out=ssq[:, c:c + 1]), r=[xk], w=['junk', ('ss', c)])
                P.dve(TS(rstd[:, c:c + 1], ssq[:, c:c + 1], 1.0 / D, EPS, ALU.mult, ALU.add), r=[('ss', c)], w=[('rs', c)])
                P.act(ACT(rstd[:, c:c + 1], rstd[:, c:c + 1], AF.Sqrt), r=[('rs', c)], w=[('rs', c)])
                P.dve(RCP(rstd[:, c:c + 1], rstd[:, c:c + 1]), r=[('rs', c)], w=[('rs', c)])
                P.dve(STT(tmp, x_, rstd[:, c:c + 1], Am, ALU.mult, ALU.mult), r=[xk, ('rs', c), 'AB'], w=['tmp'])
                h_, hk = hb.next()
                P.dve(TT(h_, tmp, Bm, ALU.add), r=['tmp', 'AB'], w=[hk])
                return h_, hk

            def n1_part2(hp_, i, h_, hk):
                hTd = hTs[hp_]
                for half in range(2):
                    pt, pk = PT.next()
                    for j in range(8):
                        kc = half * 8 + j
                        P.pe(TRP(pt[:, j * 128:(j + 1) * 128], h_[:, kc * 128:(kc + 1) * 128], ident), r=[hk], w=[pk])
                    dst = hTd[:, half * 8:half * 8 + 8, i * 128:(i + 1) * 128]
                    src = pt.rearrange("p (a b) -> p a b", b=128)
                    if half == 0:
                        P.act(ACT(dst, src, AF.Copy), r=[pk], w=[('hT', hp_, i)])
                    else:
                        P.dve(CP(dst, src), r=[pk], w=[('hT', hp_, i)])

            def evac_store(pt, pk, n, dst, func=None, scale=None):
                s_, sk = stg.next()
                if func is not None:
                    kw = {} if scale is None else {'scale': scale}
                    P.act(ACT(s_[:, :n], pt[:, :n], func, **kw), r=[pk], w=[sk])
                else:
                    evac_rr[0] += 1
                    if evac_rr[0] % 2:
                        P.act(ACT(s_[:, :n], pt[:, :n], AF.Copy), r=[pk], w=[sk])
                    else:
                        P.dve(CP(s_[:, :n], pt[:, :n]), r=[pk], w=[sk])
                P.dma('sp', dst, s_[:, :n], r=[sk], w=['dram'])

            def rope_store(pt, pk, n, toff, dst):
                q_, qk = qb.next()
                P.act(ACT(q_[:, :n], pt[:, :n], AF.Copy), r=[pk], w=[qk])
                p2, p2k = PS.next()
                P.pe(MM(p2[:, :n], pm_bf, q_[:, :n]), r=[qk], w=[p2k])
                a_, ak = t1.next(); b_, bk = t2.next()
                P.dve(TT(a_[:, :n], q_[:, :n], cosS[:, toff:toff + n], ALU.mult), r=[qk, 'cs'], w=[ak])
                P.dve(TT(b_[:, :n], p2[:, :n], sinS[:, toff:toff + n], ALU.mult), r=[p2k, 'cs'], w=[bk])
                s_, sk = stg.next()
                P.dve(TT(s_[:, :n], a_[:, :n], b_[:, :n], ALU.add), r=[ak, bk], w=[sk])
                P.dma('sp', dst, s_[:, :n], r=[sk], w=['dram'])

            def gates_store(pt, pk, dst):
                z_, zk = gz.next(); e_, ek = ge.next()
                P.dve(TT(z_, pt[:, 0:32], bg, ALU.add), r=[pk, 'bg'], w=[zk])
                z3 = z_.rearrange("p (a b) -> p a b", b=16)
                e3 = e_.rearrange("p (a b) -> p a b", b=8)
                P.act(ACT(e3, z3[:, :, 8:16], AF.Exp, scale=-1.0), r=[zk], w=[ek])
                P.act(ACT(e3, e3, AF.Ln, bias=1.0), r=[ek], w=[ek])
                P.dve(TS(z3[:, :, 8:16], e3, -1.0, None, ALU.mult), r=[ek], w=[zk])
                P.dma('sp', dst, z_, r=[zk], w=['dram'])

            def feat_gemm(wb, wk, ncol, spans, hkeys, epi):
                for cs in range(ncol // 128):
                    for si, (t0, nt) in enumerate(spans):
                        pt, pk = PS.next()
                        for kc in range(16):
                            P.pe(MM(pt[:, :nt], wb[:, kc, cs * 128:(cs + 1) * 128], hT[:, kc, t0:t0 + nt], kc == 0, kc == 15),
                                 r=[wk] + hkeys, w=[pk])
                        epi(pt, pk, cs, si, nt)

            def tok_gemm(wb, wk, ncol, tiles, epi):
                for i in tiles:
                    pt, pk = PS.next()
                    for kc in range(16):
                        P.pe(MM(pt[:, :ncol], hT[:, kc, i * 128:(i + 1) * 128], wb[:, kc, 0:ncol], kc == 0, kc == 15),
                             r=[wk, ('hT', hp, i)], w=[pk])
                    epi(pt, pk, i)

            sts = [('ctx', [(0, ctxv[0:128, :]), (1, ctxv[128:256, :])])]
            for s in range(4):
                sts.append(('pre', [(2 + s * 8 + i, xv[(s * 8 + i) * 128:(s * 8 + i + 1) * 128, :]) for i in range(8)]))
            for s in range(4):
                sts.append(('own', [(34 + s * 8 + i, xv[TOWN + (s * 8 + i) * 128:TOWN + (s * 8 + i + 1) * 128, :]) for i in range(8)]))
            if os.environ.get("MK_PSHORT"):
                sts = [sts[0], sts[4], sts[5]]
            prev_kind = None
            for sti, (kind, tl) in enumerate(sts):
                nt_ = len(tl)
                hp = sti % 2
                hT = hTs[hp]
                if sti == 0:
                    P.dma('sp', Am, modsc[6], w=['AB']); P.dma('sp', Bm, modsc[7], w=['AB'])
                    for i, (gt, src) in enumerate(tl):
                        h_, hk = n1_part1(src)
                        n1_part2(hp, i, h_, hk)
                nxt_tl = sts[sti + 1][1] if sti + 1 < len(sts) else []
                la = {'i': 0, 'pend': None}

                def lookahead(la=la, nxt_tl=nxt_tl, hp=hp, sti=sti, kind=kind):
                    if sti + 1 >= len(sts) or la['i'] > len(nxt_tl):
                        return
                    i_ = la['i']
                    if i_ == 0 and kind == 'ctx':
                        P.dma('sp', Am, modsc[0], w=['AB']); P.dma('sp', Bm, modsc[1], w=['AB'])
                    new = None
                    if i_ < len(nxt_tl):
                        new = (i_,) + n1_part1(nxt_tl[i_][1])
                    if la['pend'] is not None:
                        n1_part2(1 - hp, *la['pend'])
                    la['pend'] = new
                    la['i'] = i_ + 1
                hkeys = [('hT', hp, i) for i in range(nt_)]
                tiles = list(range(nt_))
                gts = [gt for gt, _ in tl]
                last_pre = (kind == 'pre' and gts[-1] == 33)
                if kind == 'own':
                    o0 = (gts[0] - 34) * 128
                    P.dma('sp', cosS, cosT[:, 128 + o0:128 + o0 + TS_], w=['cs'])
                    P.dma('sp', sinS, sinT[:, 128 + o0:128 + o0 + TS_], w=['cs'])
                elif last_pre:
                    P.dma('sp', cosS[:, 0:128], cosT[:, 0:128], w=['cs'])
                    P.dma('sp', sinS[:, 0:128], sinT[:, 0:128], w=['cs'])
                blocks = []
                if kind == 'own':
                    spans = [(0, 512), (512, 512)]

                    def mk_feat_epi(dst_d, mode, scale=None):
                        def epi(pt, pk, cs, si, nt, cb=None):
                            pass
                        return epi
                    blocks.append((C_AK, 512, 'akT'))
                    blocks.append((C_AV, 512, 'av'))
                    for j in range(2):
                        blocks.append((C_MK + j * 512, 512, ('mk', j)))
                    for j in range(4):
                        blocks.append((C_MV + j * 512, 512, ('mv', j)))
                    for j in range(4):
                        blocks.append((C_AQ + j * 512, 512, ('aqT', j)))
                    for j in range(2):
                        blocks.append((C_MQ + j * 512, 512, ('mqT', j)))
                    for j in range(4):
                        blocks.append((C_MO + j * 512, 512, ('mo', j)))
                    for j in range(4):
                        blocks.append((C_GA + j * 512, 512, ('sgaT', j)))
                    for j in range(4):
                        blocks.append((C_GM + j * 512, 512, ('sgmT', j)))
                elif kind == 'pre':
                    if last_pre:
                        blocks.append((C_AK, 512, 'akT'))
                        blocks.append((C_AV, 512, 'av'))
                    for j in range(2):
                        blocks.append((C_MK + j * 512, 512, ('mk', j)))
                    for j in range(4):
                        blocks.append((C_MV + j * 512, 512, ('mv', j)))
                else:
                    blocks.append((C_AK, 512, 'akT'))
                    blocks.append((C_AV, 512, 'av'))
                    for j in range(2):
                        blocks.append((C_MK + j * 512, 512, ('mk', j)))
                    for j in range(4):
                        blocks.append((C_MV + j * 512, 512, ('mv', j)))
                ws = WStream(WB, [(vin, 0, 16, c0, ncol) for (c0, ncol, _) in blocks], rk=wqkeys['in'], conv_per=1)
                def gates_epi(pt, pk, i):
                    gates_store(pt, pk, gs_d[gts[i]])
                for i in tiles:
                    pt, pk = PS.next()
                    for kc in range(16):
                        P.pe(MM(pt[:, 0:32], hT[:, kc, i * 128:(i + 1) * 128], wg_bf[:, kc, :], kc == 0, kc == 15),
                             r=['wg', ('hT', hp, i)], w=[pk])
                    gates_epi(pt, pk, i)
                for bi, (c0, ncol, use) in enumerate(blocks):
                    wb, wk = ws.get(bi)
                    lookahead()
                    name = use if isinstance(use, str) else use[0]
                    j = 0 if isinstance(use, str) else use[1]
                    if name == 'akT':
                        if kind == 'own':
                            def epi(pt, pk, cs, si, nt):
                                o0_ = (gts[0] - 34) * 128 + si * 512
                                rope_store(pt, pk, nt, si * 512, akT_d[cs, :, 128 + o0_:128 + o0_ + nt])
                            feat_gemm(wb, wk, 512, spans, hkeys, epi)
                        elif kind == 'pre':
                            def epi(pt, pk, cs, si, nt):
                                rope_store(pt, pk, nt, 0, akT_d[cs, :, 0:128])
                            feat_gemm(wb, wk, 512, [(7 * 128, 128)], [('hT', hp, 7)], epi)
                        else:
                            def epi(pt, pk, cs, si, nt):
                                evac_store(pt, pk, nt, akT_d[cs, :, TOWN + 128:TOWN + 128 + CTXL])
                            feat_gemm(wb, wk, 512, [(0, 256)], hkeys, epi)
                    elif name == 'av':
                        if kind == 'own':
                            def epi(pt, pk, i):
                                evac_store(pt, pk, 512, av_d[1 + gts[i] - 34])
                            tok_gemm(wb, wk, 512, tiles, epi)
                        elif kind == 'pre':
                            def epi(pt, pk, i):
                                evac_store(pt, pk, 512, av_d[0])
                            tok_gemm(wb, wk, 512, [7], epi)
                        else:
                            def epi(pt, pk, i):
                                evac_store(pt, pk, 512, av_d[33 + i])
                            tok_gemm(wb, wk, 512, tiles, epi)
                    elif name == 'mk':
                        def epi(pt, pk, i, j=j):
                            evac_store(pt, pk, 512, mk_d[gts[i], :, j * 512:(j + 1) * 512])
                        tok_gemm(wb, wk, 512, tiles, epi)
                        if kind == 'own':
                            def epi(pt, pk, cs, si, nt, j=j):
                                o0_ = (gts[0] - 34) * 128 + si * 512
                                evac_store(pt, pk, nt, mkT_d[j * 4 + cs, :, o0_:o0_ + nt])
                            feat_gemm(wb, wk, 512, spans, hkeys, epi)
                    elif name == 'mv':
                        def epi(pt, pk, i, j=j):
                            evac_store(pt, pk, 512, mv_d[gts[i], :, j * 512:(j + 1) * 512])
                        tok_gemm(wb, wk, 512, tiles, epi)
                    elif name == 'mo':
                        def epi(pt, pk, i, j=j):
                            evac_store(pt, pk, 512, mo_d[gts[i] - 34, :, j * 512:(j + 1) * 512], func=AF.Sigmoid)
                        tok_gemm(wb, wk, 512, tiles, epi)
                    elif name == 'aqT':
                        def epi(pt, pk, cs, si, nt, j=j):
                            o0_ = (gts[0] - 34) * 128 + si * 512
                            rope_store(pt, pk, nt, si * 512, aqT_d[j * 4 + cs, :, o0_:o0_ + nt])
                        feat_gemm(wb, wk, 512, spans, hkeys, epi)
                    elif name == 'mqT':
                        def epi(pt, pk, cs, si, nt, j=j):
                            o0_ = (gts[0] - 34) * 128 + si * 512
                            evac_store(pt, pk, nt, mqT_d[j * 4 + cs, :, o0_:o0_ + nt], func=AF.Copy, scale=128.0 ** -0.5)
                        feat_gemm(wb, wk, 512, spans, hkeys, epi)
                    elif name in ('sgaT', 'sgmT'):
                        dd = sgaT_d if name == 'sgaT' else sgmT_d
                        def epi(pt, pk, cs, si, nt, j=j, dd=dd):
                            o0_ = (gts[0] - 34) * 128 + si * 512
                            evac_store(pt, pk, nt, dd[j * 4 + cs, :, o0_:o0_ + nt], func=AF.Sigmoid)
                        feat_gemm(wb, wk, 512, spans, hkeys, epi)
                for _ in range(12):
                    lookahead()
            P.fence()


        if 'T' in PHASES:
            cv = Carve(big)
            KT = cv.bf(4 * 4480, 4480)
            V = cv.bf(35 * 512, 512)
            sinkE = cv.f32(D)
            QT = Rot([cv.bf(D) for _ in range(3)], 'QT')
            PTs = Rot([cv.bf(512) for _ in range(12)], 'PTs')
            dn = Rot([cv.f32(512) for _ in range(2)], 'dn')
            ost = Rot([cv.bf(512) for _ in range(2)], 'ost')
            for g in range(4):
                P.dma('sp', KT[:, g, :], akT_d[g], w=['KT'])
            P.dma('sp', V, av_d.rearrange("t p c -> p t c"), w=['V'])
            P.dma('sp', sinkE, sinkrep, w=['sk'])
            P.act(ACT(sinkE, sinkE, AF.Exp), r=['sk'], w=['sk'])
            PSs = Rot(ps[0:4], 'pss'); PSo = Rot(ps[4:6], 'pso'); PSd = Rot(ps[6:8], 'psd')
            NJ = int(os.environ.get("MK_TN", "32"))
            its = [(j, g) for j in range(NJ) for g in range(4)]
            qcur = {}

            def stageA(j, g):
                if g == 0:
                    q_, qk = QT.next()
                    P.dma('sp', q_.rearrange("p (h q) -> p h q", q=128),
                          aqT_d[:, :, j * 128:(j + 1) * 128].rearrange("h d q -> d h q"), w=[qk])
                    qcur[j] = (q_, qk)
                q_, qk = qcur[j]
                kbs = [(j, negB4), (j + 1, None)] + ([(j + 2, negA4)] if j < 31 else []) + [(33, None), (34, None)]
                pts = []
                for (kt, msk) in kbs:
                    s_, sk_ = PSs.next()
                    P.pe(MM(s_, KT[:, g, kt * 128:(kt + 1) * 128], q_[:, g * 512:(g + 1) * 512], True, msk is None), r=['KT', qk], w=[sk_])
                    if msk is not None:
                        P.pe(MM(s_, ident, msk, False, True), r=[], w=[sk_])
                    p_, pk_ = PTs.next()
                    P.act(ACT(p_, s_, AF.Exp, scale=128.0 ** -0.5), r=[sk_], w=[pk_])
                    pts.append((kt, p_, pk_))
                return pts

            def stageB(j, g, pts):
                po, pok = PSo.next(); pd, pdk = PSd.next()
                for idx, (kt, p_, pk_) in enumerate(pts):
                    first = idx == 0; last = idx == len(pts) - 1
                    P.pe(MM(po, V[:, kt, g * 128:(g + 1) * 128], p_, first, last), r=['V', pk_], w=[pok])
                    P.pe(MM(pd, ones_bf, p_, first, last), r=[pk_], w=[pdk])
                d_, dk_ = dn.next()
                P.dve(TT(d_, pd, sinkE[:, g * 512:(g + 1) * 512], ALU.add), r=[pdk, 'sk'], w=[dk_])
                P.dve(RCP(d_, d_), r=[dk_], w=[dk_])
                o_, ok_ = ost.next()
                P.dve(TT(o_, po, d_, ALU.mult), r=[pok, dk_], w=[ok_])
                P.dma('sp', attT_d[4 * g:4 * g + 4, :, j * 128:(j + 1) * 128].rearrange("h d q -> d h q"),
                      o_.rearrange("p (h q) -> p h q", q=128), r=[ok_], w=['dram'])
            prev = stageA(*its[0])
            for ii, (j, g) in enumerate(its):
                nxt = stageA(*its[ii + 1]) if ii + 1 < len(its) else None
                stageB(j, g, prev)
                prev = nxt
            P.fence()

        if 'M' in PHASES:
            cv = Carve(big)
            G = cv.f32(66 * 32, 32)
            lfc = [cv.f32(528), cv.f32(528)]
            bb = [cv.f32(528), cv.f32(528)]
            ee = [cv.f32(528, 8), cv.f32(528, 8)]
            uu = [cv.f32(528).rearrange("p (t h o) -> p t h o", h=8, o=1) for _ in range(2)]
            th = [cv.f32(528, 8), cv.f32(528, 8)]
            dtmp = cv.f32(528)
            Cst = [cv.f32(8 * 257, 257) for _ in range(2)]
            CbfA = cv.bf(8 * 257 + 8)[:, 0:8 * 257].rearrange("p (h c) -> p h c", c=257)
            CbfB = Rot([cv.bf(8 * 257 + 8)[:, 0:8 * 257] for _ in range(2)], 'CbfB')
            mkt = Rot([cv.bf(1024) for _ in range(3)], 'mkt')
            mvt = Rot([cv.bf(2048, 256) for _ in range(3)], 'mvt')
            vEr = [Rot([cv.bf(8 * 257 + 8)[:, 0:8 * 257].rearrange("p (h c) -> p h c", c=257) for _ in range(2)], 'vE%d' % d) for d in range(2)]
            tmpE = Rot([cv.f32(257) for _ in range(3)], 'tmpE')
            qTr = Rot([cv.bf(1024, 128) for _ in range(2)], 'qTr')
            kTr = Rot([cv.bf(1024, 128) for _ in range(2)], 'kTr')
            mot = Rot([cv.bf(2048) for _ in range(2)], 'mot')
            Sb = Rot([cv.bf(128) for _ in range(2)], 'Sb')
            SmAr = Rot([cv.bf(128) for _ in range(2)], 'SmA')
            SmBr = Rot([cv.bf(128) for _ in range(2)], 'SmB')
            hbuf = cv.f32(2048); gmo = cv.f32(2048); gm = cv.f32(2048)
            hA = Rot([cv.f32(256) for _ in range(2)], 'hA')
            junk = cv.bf(256)
            memr = Rot([cv.bf(2048) for _ in range(2)], 'mem')
            memTs = Rot([cv.bf(2048, 128) for _ in range(2)], 'memTs')
            d2r = Rot([cv.f32(2) for _ in range(6)], 'd2')
            ssm = Rot([cv.f32(8) for _ in range(2)], 'ssm')
            P.dma('sp', G, gs_d.rearrange("t p c -> p t c"), w=['G'])
            P.dma('sp', gm, gmrep, w=['gm'])
            for d in range(2):
                P.dve(CP(lfc[d].rearrange("p (t h) -> p t h", h=8), G[:, :, d * 16 + 8:d * 16 + 16]), r=['G'], w=[('lfc', d)])
            PSr = Rot(ps[0:6], 'psm')
            for d in range(2):
                tri = triA_f if d == 0 else triB_f
                for hf in range(2):
                    c0 = hf * 264
                    pb, pbk = PSr.next(); pe2, pek = PSr.next()
                    P.pe(MM(pb[:, :264], tri, lfc[d][:, c0:c0 + 264]), r=[('lfc', d)], w=[pbk])
                    P.pe(MM(pe2[:, :264], ones_f, lfc[d][:, c0:c0 + 264]), r=[('lfc', d)], w=[pek])
                    thv = th[d].rearrange("p t h -> p (t h)")[:, c0:c0 + 264]
                    eev = ee[d].rearrange("p t h -> p (t h)")[:, c0:c0 + 264]
                    uuv = uu[d].rearrange("p t h o -> p (t h o)")[:, c0:c0 + 264]
                    P.act(ACT(thv, pb[:, :264], AF.Exp, scale=-1.0), r=[pbk], w=[('th', d)])
                    P.act(ACT(eev, pe2[:, :264], AF.Exp), r=[pek], w=[('ee', d)])
                    P.dve(TT(dtmp[:, c0:c0 + 264].rearrange("p (t h) -> p t h", h=8), G[:, hf * 33:hf * 33 + 33, d * 16:d * 16 + 8],
                             pb[:, :264].rearrange("p (t h) -> p t h", h=8), ALU.subtract), r=['G', pbk], w=['dtmp'])
                    P.act(ACT(uuv, dtmp[:, c0:c0 + 264], AF.Exp), r=['dtmp'], w=[('uu', d)])
            for d in range(2):
                P.dve(MEMSET(Cst[d].rearrange("p h c -> p (h c)"), 0.0), w=[('C', d, h) for h in range(8)])
            P.dve(MEMSET(CbfA.rearrange("p h c -> p (h c)"), 0.0), w=[('CbfA', h) for h in range(8)])

            def load_kv(n):
                mk_, mkk = mkt.next(); mv_, mvk = mvt.next()
                P.dma('sp', mk_, mk_d[n], w=[mkk])
                P.dma('sp', mv_.rearrange("p h c -> p (h c)"), mv_d[n], w=[mvk])
                return mk_, mkk, mv_, mvk

            def make_vE(d, n, mv_, mvk):
                v_, vk = vEr[d].next()
                P.dve(TT(v_[:, :, 0:256], mv_, uu[d][:, n].to_broadcast([128, 8, 256]), ALU.mult), r=[mvk, ('uu', d)], w=[vk])
                P.act(ACT(v_[:, :, 256:257], uu[d][:, n], AF.Copy), r=[('uu', d)], w=[vk])
                return v_, vk

            def state_update(d, n, h, mk_, mkk, v_, vk, PSc, cbf=None, cbfk=None):
                cl, clk = PSc.next()
                P.pe(MM(cl[:, :257], mk_[:, h * 128:(h + 1) * 128], v_[:, h, :]), r=[mkk, vk], w=[clk])
                t_, tk = tmpE.next()
                e_ap = ee[d][:, n, h:h + 1]
                P.act(ACT(t_, cl[:, :257], AF.Copy, scale=e_ap), r=[clk, ('ee', d)], w=[tk])
                P.dve(STT(Cst[d][:, h, :], Cst[d][:, h, :], e_ap, t_, ALU.mult, ALU.add), r=[tk, ('ee', d)], w=[('C', d, h)])
                if cbf is not None:
                    P.pool(CP(cbf[:, h, :], Cst[d][:, h, :]), r=[('C', d, h)], w=[cbfk(h)])

            full = not os.environ.get("MK_PSHORT")
            preT = list(range(2, 34)) if full else list(range(26, 34))
            ownT = list(range(34, 66)) if full else list(range(34, 42))
            for n in [0, 1] + preT:
                mk_, mkk, mv_, mvk = load_kv(n)
                v_, vk = make_vE(0, n, mv_, mvk)
                last = (n == preT[-1])
                for h in range(8):
                    state_update(0, n, h, mk_, mkk, v_, vk, PSr, CbfA if last else None, (lambda h: ('CbfA', h)))
            for n in [1, 0] + ownT[::-1]:
                mk_, mkk, mv_, mvk = load_kv(n)
                v_, vk = make_vE(1, n, mv_, mvk)
                if n >= 34:
                    cb_, cbk = CbfB.next()
                    P.pool(CP(cb_, Cst[1].rearrange("p h c -> p (h c)")), r=[('C', 1, h) for h in range(8)], w=[cbk])
                    P.dma('sp', stB_d[n - 34], cb_, r=[cbk], w=['dram_st'])
                for h in range(8):
                    state_update(1, n, h, mk_, mkk, v_, vk, PSr)
            P.fence()
            S4 = Rot([ps[0][:, i * 128:(i + 1) * 128] for i in range(4)], 'S4')
            PNA = Rot(ps[1:3], 'pna'); PNB = Rot(ps[3:5], 'pnb'); PCL = Rot(ps[5:6], 'pcl'); PTm = Rot(psb[6:8], 'ptm')
            tstate = {}

            def prologue(n):
                j = n - 34
                mk_, mkk, mv_, mvk = load_kv(n)
                q_, qk = qTr.next(); k_, kk = kTr.next(); mo_, mok = mot.next(); cb_, cbk = CbfB.next()
                P.dma('sp', q_, mqT_d[:, :, j * 128:(j + 1) * 128].rearrange("h d t -> d h t"), w=[qk])
                P.dma('sp', k_, mkT_d[:, :, j * 128:(j + 1) * 128].rearrange("h d t -> d h t"), w=[kk])
                P.dma('sp', mo_, mo_d[j], w=[mok])
                P.dma('sp', cb_, stB_d[j], r=['dram_st'], w=[cbk])
                vA, vAk = make_vE(0, n, mv_, mvk)
                vB, vBk = make_vE(1, n, mv_, mvk)
                ss_, ssk = ssm.next()
                tstate[n] = dict(mk=(mk_, mkk), q=(q_, qk), k=(k_, kk), mo=(mo_, mok), cb=(cb_, cbk), vA=(vA, vAk), vB=(vB, vBk),
                                 ss=(ss_, ssk), sm={})

            def partA(n, h):
                if n not in tstate:
                    prologue(n)
                t_ = tstate[n]
                q_, qk = t_['q']; k_, kk = t_['k']
                s_, sk_ = S4.next()
                P.pe(MM(s_, k_[:, h, :], q_[:, h, :]), r=[kk, qk], w=[sk_])
                sb_, sbk = Sb.next()
                P.act(ACT(sb_, s_, AF.Copy), r=[sk_], w=[sbk])
                sa_, sak = SmAr.next(); sB_, sBk = SmBr.next()
                P.pool(TT(sa_, sb_, triA4[:, 0:128], ALU.mult), r=[sbk], w=[sak])
                P.pool(TT(sB_, sb_, triB4[:, 0:128], ALU.mult), r=[sbk], w=[sBk])
                t_['sm'][h] = (sa_, sak, sB_, sBk)

            def partB(n, h):
                t_ = tstate[n]
                mk_, mkk = t_['mk']; q_, qk = t_['q']; cb_, cbk = t_['cb']; vA, vAk = t_['vA']; vB, vBk = t_['vB']
                ss_, ssk = t_['ss']
                sa_, sak, sB_, sBk = t_['sm'][h]
                cb3 = cb_.rearrange("p (h c) -> p h c", c=257)
                na, nak = PNA.next(); nb_, nbk = PNB.next()
                P.pe(MM(na[:, :257], sa_, vA[:, h, :], True, False), r=[sak, vAk], w=[nak])
                P.pe(MM(na[:, :257], q_[:, h, :], CbfA[:, h, :], False, True), r=[qk, ('CbfA', h)], w=[nak])
                P.pe(MM(nb_[:, :257], sB_, vB[:, h, :], True, False), r=[sBk, vBk], w=[nbk])
                P.pe(MM(nb_[:, :257], q_[:, h, :], cb3[:, h, :], False, True), r=[qk, cbk], w=[nbk])
                d2, d2k = d2r.next()
                P.dve(TS(d2[:, 0:1], na[:, 256:257], -1.0, na[:, 256:257], ALU.mult, ALU.max), r=[nak], w=[d2k])
                P.dve(TT(d2[:, 0:1], d2[:, 0:1], th[0][:, n, h:h + 1], ALU.max), r=[d2k, ('th', 0)], w=[d2k])
                P.dve(TS(d2[:, 1:2], nb_[:, 256:257], -1.0, nb_[:, 256:257], ALU.mult, ALU.max), r=[nbk], w=[d2k])
                P.dve(TT(d2[:, 1:2], d2[:, 1:2], th[1][:, n, h:h + 1], ALU.max), r=[d2k, ('th', 1)], w=[d2k])
                P.dve(RCP(d2, d2), r=[d2k], w=[d2k])
                ha_, hak = hA.next()
                P.act(ACT(ha_, na[:, 0:256], AF.Copy, scale=d2[:, 0:1]), r=[nak, d2k], w=[hak])
                P.dve(STT(hbuf[:, h * 256:(h + 1) * 256], nb_[:, 0:256], d2[:, 1:2], ha_, ALU.mult, ALU.add),
                      r=[nbk, d2k, hak], w=[('hbuf', h)])
                P.act(ACT(junk, hbuf[:, h * 256:(h + 1) * 256], AF.Square, accum_out=ss_[:, h:h + 1]), r=[('hbuf', h)], w=['junkm', ssk])
                state_update(0, n, h, mk_, mkk, vA, vAk, PCL, CbfA, (lambda h: ('CbfA', h)))

            def epilogue(n):
                j = n - 34
                t_ = tstate.pop(n)
                mo_, mok = t_['mo']; ss_, ssk = t_['ss']
                P.pool(TT(gmo, mo_, gm, ALU.mult), r=[mok, 'gm'], w=['gmo'])
                P.dve(TS(ss_, ss_, 1.0 / 256, EPS, ALU.mult, ALU.add), r=[ssk], w=[ssk])
                P.act(ACT(ss_, ss_, AF.Sqrt), r=[ssk], w=[ssk])
                P.dve(RCP(ss_, ss_), r=[ssk], w=[ssk])
                m_, mk2 = memr.next()
                for h in range(8):
                    P.dve(STT(m_[:, h * 256:(h + 1) * 256], hbuf[:, h * 256:(h + 1) * 256], ss_[:, h:h + 1],
                              gmo[:, h * 256:(h + 1) * 256], ALU.mult, ALU.mult), r=[('hbuf', h), ssk, 'gmo'], w=[mk2])
                mt_, mtk = memTs.next()
                for half in range(2):
                    pt, pk = PTm.next()
                    for jj in range(8):
                        kc = half * 8 + jj
                        P.pe(TRP(pt[:, jj * 128:(jj + 1) * 128], m_[:, kc * 128:(kc + 1) * 128], ident), r=[mk2], w=[pk])
                    P.act(ACT(mt_[:, half * 8:half * 8 + 8, :], pt.rearrange("p (a b) -> p a b", b=128), AF.Copy), r=[pk], w=[mtk])
                P.dma('sp', memT_d[:, :, j * 128:(j + 1) * 128].rearrange("c p t -> p c t"), mt_, r=[mtk], w=['dram'])
            seq = [(n, h) for n in ownT for h in range(8)]
            partA(*seq[0])
            for ii, (n, h) in enumerate(seq):
                if ii + 1 < len(seq):
                    partA(*seq[ii + 1])
                partB(n, h)
                if h == 7:
                    epilogue(n)
            P.fence()

        if 'G' in PHASES:
            cv = Carve(big)
            attT = cv.bf(16 * 512, 512); memT = cv.bf(16 * 512, 512)
            x2b = big[:, 0:4 * D].rearrange("p (i c) -> p i c", c=D)
            yT = cv.bf(16 * 512, 512)
            sgr = Rot([cv.bf(512) for _ in range(4)], 'sg')
            tmp = cv.f32(D)
            hb = Rot([cv.bf(D) for _ in range(2)], 'hb')
            junk = cv.bf(D)
            G1 = cv.f32(D); A2 = cv.f32(D); B2 = cv.f32(D)
            WB = [cv.bf(16 * 512, 512) for _ in range(4)]
            h2Ts = Rot([cv.bf(2048, 128) for _ in range(2)], 'h2Ts')
            t1 = Rot([cv.f32(512) for _ in range(2)], 't1')
            t2 = Rot([cv.f32(512) for _ in range(2)], 't2')
            xp = Rot([cv.f32(512) for _ in range(3)], 'xp')
            ssq = Rot([cv.f32(1) for _ in range(4)], 'ssq')
            P.dma('sp', G1, modsc[2], w=['G1']); P.dma('sp', A2, modsc[3], w=['A2']); P.dma('sp', B2, modsc[4], w=['B2'])
            vap = wview(q_ap); vmp = wview(q_mp); vo = wview(q_o)
            PS = Rot(ps[0:6], 'ps'); PT = Rot(psb[6:8], 'pt')
            nst = int(os.environ.get("MK_GN", "8"))
            blocks = []
            for st in range(nst):
                for fg in range(4):
                    blocks.append((vap, 0, 16, fg * 512, 512)); blocks.append((vmp, 0, 16, fg * 512, 512))
                for nb in range(4):
                    blocks.append((vo, 0, 16, nb * 512, 512))
            conv_issue(1000)
            ws = WStream(WB, blocks, rk=wqkeys['ap'] + wqkeys['mp'] + wqkeys['o'])
            for st in range(nst):
                o0 = st * 512
                P.dma('sp', attT, attT_d[:, :, o0:o0 + 512].rearrange("c p t -> p c t"), w=[('x2', 0), ('x2', 1)])
                P.dma('sp', memT, memT_d[:, :, o0:o0 + 512].rearrange("c p t -> p c t"), w=[('x2', 2), ('x2', 3)])
                for fg in range(4):
                    wa, wak = ws.get(st * 12 + 2 * fg, 2); wm, wmk = ws.get(st * 12 + 2 * fg + 1, 2)
                    for cs in range(4):
                        fc = fg * 4 + cs
                        pa, pak = PS.next(); pm2, pmk = PS.next()
                        for kc in range(16):
                            P.pe(MM(pa, wa[:, kc, cs * 128:(cs + 1) * 128], attT[:, kc, :], kc == 0, kc == 15), r=[wak, ('x2', 0), ('x2', 1)], w=[pak])
                        for kc in range(16):
                            P.pe(MM(pm2, wm[:, kc, cs * 128:(cs + 1) * 128], memT[:, kc, :], kc == 0, kc == 15), r=[wmk, ('x2', 2), ('x2', 3)], w=[pmk])
                        sa_, sak = sgr.next(); sm_, smk = sgr.next()
                        P.dma('sp', sa_, sgaT_d[fc, :, o0:o0 + 512], w=[sak])
                        P.dma('sp', sm_, sgmT_d[fc, :, o0:o0 + 512], w=[smk])
                        a_, ak = t1.next(); b_, bk = t2.next()
                        P.dve(TT(a_, pa, sa_, ALU.mult), r=[pak, sak], w=[ak])
                        P.dve(TT(b_, pm2, sm_, ALU.mult), r=[pmk, smk], w=[bk])
                        P.dve(TT(yT[:, fc, :], a_, b_, ALU.add), r=[ak, bk], w=[('yT', fc)])
                ykeys = [('yT', fc) for fc in range(16)]
                for nb in range(4):
                    wo, wok = ws.get(st * 12 + 8 + nb)
                    for i in range(4):
                        pt, pk = PS.next()
                        for kc in range(16):
                            P.pe(MM(pt, yT[:, kc, i * 128:(i + 1) * 128], wo[:, kc, :], kc == 0, kc == 15), r=[wok] + ykeys, w=[pk])
                        x_, xk = xp.next()
                        tok0 = TOWN + o0 + i * 128
                        P.dma('sp', x_, xv[tok0:tok0 + 128, nb * 512:(nb + 1) * 512], w=[xk])
                        a_, ak = t1.next()
                        P.dve(TT(a_, pt, G1[:, nb * 512:(nb + 1) * 512], ALU.mult), r=[pk, 'G1'], w=[ak])
                        P.dve(TT(x2b[:, i, nb * 512:(nb + 1) * 512], a_, x_, ALU.add), r=[ak, xk], w=[('x2', i)])
                for i in range(4):
                    gt = st * 4 + i
                    P.dma('sp', x2_d[gt], x2b[:, i, :], r=[('x2', i)], w=['dram'])
                    s1, s1k = ssq.next()
                    P.act(ACT(junk, x2b[:, i, :], AF.Square, accum_out=s1), r=[('x2', i)], w=['junk', s1k])
                    P.dve(TS(s1, s1, 1.0 / D, EPS, ALU.mult, ALU.add), r=[s1k], w=[s1k])
                    P.act(ACT(s1, s1, AF.Sqrt), r=[s1k], w=[s1k])
                    P.dve(RCP(s1, s1), r=[s1k], w=[s1k])
                    P.dve(STT(tmp, x2b[:, i, :], s1, A2, ALU.mult, ALU.mult), r=[('x2', i), s1k, 'A2'], w=['tmp'])
                    h_, hk = hb.next()
                    P.dve(TT(h_, tmp, B2, ALU.add), r=['tmp', 'B2'], w=[hk])
                    ht_, htk = h2Ts.next()
                    for half in range(2):
                        pt, pk = PT.next()
                        for jj in range(8):
                            kc = half * 8 + jj
                            P.pe(TRP(pt[:, jj * 128:(jj + 1) * 128], h_[:, kc * 128:(kc + 1) * 128], ident), r=[hk], w=[pk])
                        P.act(ACT(ht_[:, half * 8:half * 8 + 8, :], pt.rearrange("p (a b) -> p a b", b=128), AF.Copy), r=[pk], w=[htk])
                    P.dma('sp', h2T_d[:, :, gt * 128:(gt + 1) * 128].rearrange("c p t -> p c t"), ht_, r=[htk], w=['dram'])
            P.fence()

        if 'F' in PHASES:
            cv = Carve(big)
            h2T = cv.bf(16 * 512, 512)
            actT = cv.bf(44 * 512, 512)
            WB = [cv.bf(16 * 512, 512) for _ in range(4)]
            x3 = cv.f32(4 * D, D)
            G2 = cv.f32(D); gF = cv.f32(D)
            t1 = Rot([cv.f32(512) for _ in range(2)], 't1')
            junk = cv.bf(D)
            ssq = Rot([cv.f32(1) for _ in range(4)], 'ssq')
            P.dma('sp', G2, modsc[5], w=['G2']); P.dma('sp', gF, gfrep, w=['gF'])
            vfi = wview(q_fi); vfo = wview(q_fo)
            PS = Rot(ps[0:4], 'ps')
            nst = int(os.environ.get("MK_GN", "8"))
            blocks = []
            for st in range(nst):
                for fg in range(11):
                    blocks.append((vfi, 0, 16, fg * 512, 512)); blocks.append((vfi, 0, 16, DFF + fg * 512, 512))
                for nb in range(4):
                    for kg in range(4):
                        blocks.append((vfo, kg * 11, 11, nb * 512, 512))
            conv_issue(1000)
            ws = WStream(WB, blocks, rk=wqkeys['fi'] + wqkeys['fo'])
            for st in range(nst):
                o0 = st * 512
                P.dma('sp', h2T, h2T_d[:, :, o0:o0 + 512].rearrange("c p t -> p c t"), w=['h2T'])
                for i in range(4):
                    P.dma('sp', x3[:, i, :], x2_d[st * 4 + i], w=[('x3', i)])
                for fg in range(11):
                    wg_, wgk = ws.get(st * 38 + 2 * fg, 2); wu_, wuk = ws.get(st * 38 + 2 * fg + 1, 2)
                    for cs in range(4):
                        fc = fg * 4 + cs
                        pg, pgk = PS.next(); pu, puk = PS.next()
                        for kc in range(16):
                            P.pe(MM(pg, wg_[:, kc, cs * 128:(cs + 1) * 128], h2T[:, kc, :], kc == 0, kc == 15), r=[wgk, 'h2T'], w=[pgk])
                        for kc in range(16):
                            P.pe(MM(pu, wu_[:, kc, cs * 128:(cs + 1) * 128], h2T[:, kc, :], kc == 0, kc == 15), r=[wuk, 'h2T'], w=[puk])
                        a_, ak = t1.next()
                        P.act(ACT(a_, pg, AF.Silu), r=[pgk], w=[ak])
                        P.dve(TT(actT[:, fc, :], a_, pu, ALU.mult), r=[ak, puk], w=[('actT', fc)])
                akeys = [('actT', fc) for fc in range(44)]
                for nb in range(4):
                    for kg in range(4):
                        wo, wok = ws.get(st * 38 + 22 + nb * 4 + kg)
                        for i in range(4):
                            for k in range(11):
                                kc = kg * 11 + k
                                P.pe(MM(ps[4 + i], actT[:, kc, i * 128:(i + 1) * 128], wo[:, k, :], kg == 0 and k == 0, kg == 3 and k == 10),
                                     r=[wok] + akeys, w=[('acc', i)])
                    for i in range(4):
                        a_, ak = t1.next()
                        P.dve(TT(a_, ps[4 + i], G2[:, nb * 512:(nb + 1) * 512], ALU.mult), r=[('acc', i), 'G2'], w=[ak])
                        P.dve(TT(x3[:, i, nb * 512:(nb + 1) * 512], a_, x3[:, i, nb * 512:(nb + 1) * 512], ALU.add), r=[ak, ('x3', i)], w=[('x3', i)])
                for i in range(4):
                    gt = st * 4 + i
                    s1, s1k = ssq.next()
                    P.act(ACT(junk, x3[:, i, :], AF.Square, accum_out=s1), r=[('x3', i)], w=['junk', s1k])
                    P.dve(TS(s1, s1, 1.0 / D, EPS, ALU.mult, ALU.add), r=[s1k], w=[s1k])
                    P.act(ACT(s1, s1, AF.Sqrt), r=[s1k], w=[s1k])
                    P.dve(RCP(s1, s1), r=[s1k], w=[s1k])
                    P.dve(STT(x3[:, i, :], x3[:, i, :], s1, gF, ALU.mult, ALU.mult), r=[('x3', i), s1k, 'gF'], w=[('x3', i)])
                    P.dma('sp', out[gt * 128:(gt + 1) * 128, :], x3[:, i, :], r=[('x3', i)], w=['out'])
            P.fence()

        P.resolve(sems, dsems)
        with nc.Block() as block:
            @block.tensor
            def _(e):
                P.emit('pe', e)

            @block.scalar
            def _(e):
                P.emit('act', e)

            @block.vector
            def _(e):
                P.emit('dve', e)

            @block.gpsimd
            def _(e):
                P.emit('pool', e)

            @block.sync
            def _(e):
                P.emit('sp', e, final_wait=True)
    return nc


def _rep(v, n=128):
    return np.ascontiguousarray(np.broadcast_to(np.asarray(v, np.float32).reshape(1, -1), (n, v.size)))


def make_in_maps(x, c, ctx, c_ctx, w_ada, b_ada, norm1_g, w_in, b_gates, attn_sink, mlstm_norm_g,
                 w_attn_proj, w_mlstm_proj, w_out, norm2_g, w_ffn_in, w_ffn_out, final_norm_g):
    f = np.float32
    x = np.asarray(x, f); ctx = np.asarray(ctx, f); c = np.asarray(c, f); c_ctx = np.asarray(c_ctx, f)
    W = dict(w_ada=np.ascontiguousarray(np.asarray(w_ada, f)[0]), w_in=np.ascontiguousarray(np.asarray(w_in, f)[0]),
             w_ap=np.ascontiguousarray(np.asarray(w_attn_proj, f)[0]), w_mp=np.ascontiguousarray(np.asarray(w_mlstm_proj, f)[0]),
             w_o=np.ascontiguousarray(np.asarray(w_out, f)[0]), w_fi=np.ascontiguousarray(np.asarray(w_ffn_in, f)[0]),
             w_fo=np.ascontiguousarray(np.asarray(w_ffn_out, f)[0]))
    shared = dict(W)
    shared['bada_rep'] = _rep(np.asarray(b_ada, f)[0])
    shared['g1rep'] = _rep(np.asarray(norm1_g, f)[0]); shared['g2rep'] = _rep(np.asarray(norm2_g, f)[0])
    shared['gfrep'] = _rep(np.asarray(final_norm_g, f)); shared['gmrep'] = _rep(np.asarray(mlstm_norm_g, f)[0])
    shared['sinkrep'] = _rep(np.repeat(np.asarray(attn_sink, f)[0], 128))
    shared['cident'] = np.eye(128, dtype=f)
    pm = np.zeros((128, 128), f)
    for j in range(128):
        partner = j + 32 if (j % 64) < 32 else j - 32
        pm[partner, j] = 1.0
    shared['cpm'] = pm
    s_ = np.arange(128)
    triA = (s_[:, None] <= s_[None, :]).astype(f); triB = (s_[:, None] >= s_[None, :]).astype(f)
    shared['ctriA'] = np.ascontiguousarray(np.tile(triA, (1, 4))); shared['ctriB'] = np.ascontiguousarray(np.tile(triB, (1, 4)))
    shared['cones'] = np.ones((128, 128), f)
    shared['cnegA'] = np.ascontiguousarray(np.tile(np.where(triA > 0, 0.0, -30000.0).astype(f), (1, 4)))
    shared['cnegB'] = np.ascontiguousarray(np.tile(np.where(triB > 0, 0.0, -30000.0).astype(f), (1, 4)))

    def crepf(v):
        return np.ascontiguousarray(np.broadcast_to(v.reshape(16, 128).T[:, :, None], (128, 16, 128)).reshape(128, D))
    shared['cctxrep'] = crepf(c_ctx)
    wg_full = W['w_in'][:, C_MG:C_MG + 32]
    bgf = np.asarray(b_gates, f)[0]
    inv_freq = (np.float32(10000.0) ** (-np.arange(32, dtype=f) / np.float32(32))).astype(f)
    jj = np.arange(128)
    in_maps = []
    for core in range(8):
        b, half = core // 2, core % 2
        flip = (half == 0)
        m = dict(shared)
        m['xv'] = np.ascontiguousarray(x[b][::-1]) if flip else np.ascontiguousarray(x[b])
        m['ctxv'] = np.ascontiguousarray(ctx[b][::-1]) if flip else np.ascontiguousarray(ctx[b])
        m['crep'] = crepf(c[b])
        dA = 1 if flip else 0
        order = list(range(dA * 16, dA * 16 + 16)) + list(range((1 - dA) * 16, (1 - dA) * 16 + 16))
        m['w_g'] = np.ascontiguousarray(wg_full[:, order]); m['bg_rep'] = _rep(bgf[order])
        tpos = np.arange(TOWN - 128, SEQ)
        torig = (SEQ - 1 - tpos) if flip else tpos
        rows = (torig // 64).astype(f); cols = (torig % 64).astype(f)
        pos = np.where((jj < 64)[:, None], rows[None, :], cols[None, :]).astype(f)
        ang = (pos * inv_freq[jj % 32][:, None]).astype(f)
        sgn = np.where((jj % 64) < 32, -1.0, 1.0).astype(f)[:, None]
        m['cosT'] = np.ascontiguousarray(np.cos(ang).astype(f)); m['sinT'] = np.ascontiguousarray((np.sin(ang) * sgn).astype(f))
        in_maps.append(m)
    return in_maps


def kernel(**inputs):
    in_maps = make_in_maps(**inputs)
    nc = build()
    res = run_bass_kernel_spmd(nc, in_maps, core_ids=list(range(8)))
    outp = np.empty((4, SEQ, D), np.float32)
    for core in range(8):
        b, half = core // 2, core % 2
        o = np.asarray(res.results[core]["out"], np.float32)
        if half == 0:
            outp[b, 0:TOWN] = o[::-1]
        else:
            outp[b, TOWN:SEQ] = o
    return outp
```
